# Optimizing a Trainium2 kernel written in Bass

```python
import functools
import jax, jax.numpy as jnp
from jax import lax
import numpy as np

D_MODEL = 1024
BATCH = 4
SEQ = 4096
DEPTH = 1
DEC_BATCH = 128
DEC_SEQ = 1
PAST_LEN = 8192
PAGE_SIZE = 128

RW_HEADS = 8
RW_HEAD_DIM = 64
RW_WIDTH = RW_HEADS * RW_HEAD_DIM
DECAY_LORA = 64
ICLR_LORA = 64
GATE_LORA = 128
RW_COLS = 3 * RW_WIDTH + DECAY_LORA + ICLR_LORA + GATE_LORA
GN_EPS = 64e-5

MLA_HEADS = 8
QK_NOPE = 64
QK_ROPE = 32
V_HEAD = 64
Q_LORA = 384
KV_LORA = 256
MLA_COLS = Q_LORA + KV_LORA + QK_ROPE
ROPE_THETA = 10000.0
Q_BLOCK = 128
ATTN_SCALE = (QK_NOPE + QK_ROPE) ** -0.5

IN_COLS = RW_COLS + MLA_COLS + 2 * D_MODEL

N_EXPERTS = 64
TOP_K = 8
N_GROUPS = 8
TOPK_GROUPS = 4
EXPERT_FF = 256
SHARED_FF = 256
ROUTED_SCALE = 2.5
MOE_BLOCK = 128

DN_ALPHA = (2 * DEPTH) ** 0.25
DN_BETA = (8 * DEPTH) ** -0.25
LN_EPS = 1e-5
RMS_EPS = 1e-6

kernel_name = 'hybrid_rwkv7_mla_moe_decode_step'


def _layernorm(x, g, b):
    xf = x.astype(jnp.float32)
    mu = xf.mean(-1, keepdims=True)
    var = jnp.square(xf - mu).mean(-1, keepdims=True)
    return ((xf - mu) * lax.rsqrt(var + LN_EPS) * g + b).astype(x.dtype)


def _rmsnorm(x, g):
    xf = x.astype(jnp.float32)
    return (xf * lax.rsqrt(jnp.mean(xf * xf, -1, keepdims=True) + RMS_EPS) * g).astype(x.dtype)


def _rope(x, pos):
    half = QK_ROPE // 2
    inv = ROPE_THETA ** (-jnp.arange(half, dtype=jnp.float32) / half)
    ang = pos.astype(jnp.float32)[:, None] * inv[None, :]
    cos = jnp.cos(ang)[None, :, None, :]
    sin = jnp.sin(ang)[None, :, None, :]
    xf = x.astype(jnp.float32)
    x1, x2 = xf[..., :half], xf[..., half:]
    return jnp.concatenate([x1 * cos - x2 * sin, x1 * sin + x2 * cos], -1).astype(x.dtype)


def _swiglu(x, wg, wu, wd):
    return (jax.nn.silu(x @ wg) * (x @ wu)) @ wd


def _rwkv7(pm, wkv0, P):
    bsz, t, _ = pm.shape
    o1, o2, o3 = RW_WIDTH, 2 * RW_WIDTH, 3 * RW_WIDTH
    o4, o5 = o3 + DECAY_LORA, o3 + DECAY_LORA + ICLR_LORA
    r, k, v, xw, xa, xg = jnp.split(pm, [o1, o2, o3, o4, o5], axis=-1)
    w_log = -jax.nn.softplus(-(P['w_decay0'] + jnp.tanh(xw) @ P['w_decay_up']).astype(jnp.float32)) - 0.5
    decay = jnp.exp(-jnp.exp(w_log))
    a = jax.nn.sigmoid((P['a0'] + xa @ P['w_iclr_up']).astype(jnp.float32))
    g = jax.nn.sigmoid(xg) @ P['w_gate_up']
    hd = lambda z: z.astype(jnp.float32).reshape(bsz, t, RW_HEADS, RW_HEAD_DIM)
    hp = lambda z: z.astype(jnp.float32).reshape(RW_HEADS, RW_HEAD_DIM)
    kk = hd(k * P['k_k'])
    kk = kk * lax.rsqrt(jnp.sum(kk * kk, -1, keepdims=True) + 1e-12)
    k_mod = hd(k) * (1.0 + (hd(a) - 1.0) * hp(P['k_a']))
    r_h, v_h, a_h, w_h = hd(r), hd(v), hd(a), hd(decay)

    def step(S, inp):
        r_t, k_t, v_t, kk_t, a_t, w_t = inp
        sa = jnp.einsum('bhvk,bhk->bhv', S, -kk_t)
        S = (S * w_t[:, :, None, :] + sa[..., None] * (kk_t * a_t)[:, :, None, :]
             + v_t[..., None] * k_t[:, :, None, :])
        return S, jnp.einsum('bhvk,bhk->bhv', S, r_t)

    xs = tuple(jnp.moveaxis(z, 1, 0) for z in (r_h, k_mod, v_h, kk, a_h, w_h))
    S_T, o = lax.scan(step, wkv0.astype(jnp.float32), xs)
    o = jnp.moveaxis(o, 0, 1)
    mu = o.mean(-1, keepdims=True)
    var = jnp.square(o - mu).mean(-1, keepdims=True)
    o = ((o - mu) * lax.rsqrt(var + GN_EPS)).reshape(bsz, t, RW_WIDTH) * P['lnx_g'] + P['lnx_b']
    bonus = jnp.sum(r_h * k_mod * hp(P['r_k']), -1, keepdims=True) * v_h
    o = (o + bonus.reshape(bsz, t, RW_WIDTH)) * g
    return o.astype(pm.dtype) @ P['w_branch_a'], S_T


def _mla_prompt(q_nope, q_rope, ckv, k_rope, w_uk, w_uv):
    bsz, t = q_nope.shape[0], q_nope.shape[1]
    k_nope = jnp.einsum('btc,chd->bthd', ckv, w_uk.reshape(KV_LORA, MLA_HEADS, QK_NOPE))
    v = jnp.einsum('btc,chd->bthd', ckv, w_uv.reshape(KV_LORA, MLA_HEADS, V_HEAD))
    qb = min(Q_BLOCK, t)
    nb = t // qb
    qn = q_nope.reshape(bsz, nb, qb, MLA_HEADS, QK_NOPE).swapaxes(0, 1)
    qr = q_rope.reshape(bsz, nb, qb, MLA_HEADS, QK_ROPE).swapaxes(0, 1)
    kpos = jnp.arange(t)

    def block(args):
        qn_b, qr_b, i = args
        s = (jnp.einsum('bqhd,bkhd->bhqk', qn_b, k_nope)
             + jnp.einsum('bqhr,bkr->bhqk', qr_b, k_rope)).astype(jnp.float32) * ATTN_SCALE
        qpos = i * qb + jnp.arange(qb)
        s = jnp.where(kpos[None, :] <= qpos[:, None], s, -jnp.inf)
        p = jax.nn.softmax(s, axis=-1).astype(v.dtype)
        return jnp.einsum('bhqk,bkhd->bqhd', p, v)

    o = lax.map(block, (qn, qr, jnp.arange(nb)))
    return o.swapaxes(0, 1).reshape(bsz, t, MLA_HEADS, V_HEAD)


def _mla_sample(past_ckv, past_kr, q_nope, q_rope, ckv, k_rope, w_uk, w_uv):
    past = past_ckv.shape[1]
    s_len = q_nope.shape[1]
    w_uk3 = w_uk.reshape(KV_LORA, MLA_HEADS, QK_NOPE)
    q_lat = jnp.einsum('bshd,chd->bshc', q_nope, w_uk3)
    s_past = (jnp.einsum('bshc,btc->bhst', q_lat, past_ckv)
              + jnp.einsum('bshr,btr->bhst', q_rope, past_kr))
    s_new = (jnp.einsum('bshc,buc->bhsu', q_lat, ckv)
             + jnp.einsum('bshr,bur->bhsu', q_rope, k_rope))
    causal = jnp.arange(s_len)[None, :] <= jnp.arange(s_len)[:, None]
    s_new = jnp.where(causal, s_new.astype(jnp.float32), -jnp.inf)
    s = jnp.concatenate([s_past.astype(jnp.float32), s_new], -1) * ATTN_SCALE
    p = jax.nn.softmax(s, axis=-1).astype(ckv.dtype)
    o_lat = (jnp.einsum('bhst,btc->bshc', p[..., :past], past_ckv)
             + jnp.einsum('bhsu,buc->bshc', p[..., past:], ckv))
    return jnp.einsum('bshc,chd->bshd', o_lat, w_uv.reshape(KV_LORA, MLA_HEADS, V_HEAD))


def _routed_experts(h, e_idx, wts, w_g, w_u, w_d):
    n = h.shape[0]
    a = n * TOP_K
    flat_e = e_idx.reshape(a)
    order = jnp.argsort(flat_e)
    e_sorted = flat_e[order]
    tok_sorted = (order // TOP_K).astype(jnp.int32)
    counts = jnp.zeros((N_EXPERTS,), jnp.int32).at[flat_e].add(1)
    padded = (counts + MOE_BLOCK - 1) // MOE_BLOCK * MOE_BLOCK
    pad_end = jnp.cumsum(padded)
    pad_start = pad_end - padded
    start = jnp.cumsum(counts) - counts
    dest = pad_start[e_sorted] + jnp.arange(a, dtype=jnp.int32) - start[e_sorted]
    n_blocks = -(-a // MOE_BLOCK) + N_EXPERTS
    slot_tok = jnp.full((n_blocks * MOE_BLOCK,), n, jnp.int32).at[dest].set(tok_sorted)
    block_e = jnp.minimum(jnp.searchsorted(pad_end, jnp.arange(n_blocks, dtype=jnp.int32) * MOE_BLOCK,
                                           side='right'), N_EXPERTS - 1)
    h_ext = jnp.concatenate([h, jnp.zeros((1, h.shape[1]), h.dtype)], 0)
    xb = h_ext[slot_tok].reshape(n_blocks, MOE_BLOCK, h.shape[1])

    def one_block(args):
        xblk, e = args
        return _swiglu(xblk, w_g[e], w_u[e], w_d[e])

    yb = lax.map(one_block, (xb, block_e)).reshape(n_blocks * MOE_BLOCK, h.shape[1])
    y_assign = (yb[dest] * wts.reshape(a)[order][:, None]).astype(h.dtype)
    return jnp.zeros_like(h).at[tok_sorted].add(y_assign)


def _moe(h, P):
    n = h.shape[0]
    s = jax.nn.sigmoid((h @ P['w_router']).astype(jnp.float32))
    sb = s + P['b_router'].astype(jnp.float32)
    g_score = lax.top_k(sb.reshape(n, N_GROUPS, N_EXPERTS // N_GROUPS), 2)[0].sum(-1)
    _, g_idx = lax.top_k(g_score, TOPK_GROUPS)
    g_mask = jax.nn.one_hot(g_idx, N_GROUPS).sum(1) > 0
    e_mask = jnp.repeat(g_mask, N_EXPERTS // N_GROUPS, axis=1)
    _, e_idx = lax.top_k(jnp.where(e_mask, sb, -jnp.inf), TOP_K)
    wts = jnp.take_along_axis(s, e_idx, axis=1)
    wts = wts / jnp.sum(wts, -1, keepdims=True) * ROUTED_SCALE
    routed = _routed_experts(h, e_idx, wts, P['w_exp_gate'], P['w_exp_up'], P['w_exp_down'])
    return routed + _swiglu(h, P['w_sh_gate'], P['w_sh_up'], P['w_sh_down'])


def _layer(x, c, shift0, wkv0, pos, attend, P):
    bsz, t, _ = x.shape
    mod = jax.nn.silu(c) @ P['w_ada'] + P['b_ada']
    sh1, sc1, gt1, sh2, sc2, gt2 = jnp.split(mod[:, None, :], 6, axis=-1)
    h = x * (1 + sc1) + sh1
    proj = h @ P['w_in']
    o1 = RW_COLS
    o2 = o1 + Q_LORA
    o3 = o2 + KV_LORA
    o4 = o3 + QK_ROPE
    o5 = o4 + D_MODEL
    p_rw, q_low, kv_low, kr_low, gpre_a, gpre_b = jnp.split(proj, [o1, o2, o3, o4, o5], axis=-1)
    p_prev = jnp.concatenate([shift0[:, None, :].astype(p_rw.dtype), p_rw[:, :-1]], axis=1)
    p_mix = p_rw + (p_prev - p_rw) * P['mu_shift']
    y_a, wkv_new = _rwkv7(p_mix, wkv0, P)
    cq = _rmsnorm(q_low, P['g_qnorm'])
    q = (cq @ P['w_uq']).reshape(bsz, t, MLA_HEADS, QK_NOPE + QK_ROPE)
    q_nope = q[..., :QK_NOPE]
    q_rope = _rope(q[..., QK_NOPE:], pos)
    ckv = _rmsnorm(kv_low, P['g_kvnorm'])
    k_rope = _rope(kr_low[:, :, None, :], pos)[:, :, 0, :]
    o_b = attend(q_nope, q_rope, ckv, k_rope, P['w_uk'], P['w_uv'])
    y_b = o_b.reshape(bsz, t, MLA_HEADS * V_HEAD) @ P['w_branch_b']
    merged = jax.nn.sigmoid(gpre_a) * y_a + jax.nn.sigmoid(gpre_b) * y_b
    x = _layernorm(DN_ALPHA * x + gt1 * (merged @ P['w_out']), P['ln1_g'], P['ln1_b'])
    h2 = x * (1 + sc2) + sh2
    ffn = _moe(h2.reshape(bsz * t, D_MODEL), P).reshape(x.shape)
    x = _layernorm(DN_ALPHA * x + gt2 * ffn, P['ln2_g'], P['ln2_b'])
    return x, ckv, k_rope, wkv_new, p_rw[:, -1]


def setup_inputs(seed: int = 0) -> dict:
    key = jax.random.key(seed)
    ks = iter(jax.random.split(key, 64))
    f32 = jnp.float32
    L, D = DEPTH, D_MODEL

    def nrm(shape, scale):
        return jax.random.normal(next(ks), shape, f32) * scale

    def gain(shape):
        return 1.0 + nrm(shape, 0.02)

    n_pages = PAST_LEN // PAGE_SIZE
    n_used = DEC_BATCH * n_pages
    n_phys = n_used + max(1, n_used // 4)
    page_table = jax.random.permutation(next(ks), n_phys)[:n_used].reshape(DEC_BATCH, n_pages).astype(jnp.int32)
    col_scale = jnp.ones((IN_COLS,), f32).at[2 * RW_WIDTH:3 * RW_WIDTH].set(DN_BETA)
    x_prompt = nrm((BATCH, SEQ, D), 1.0)
    x_sample = nrm((DEC_BATCH, DEC_SEQ, D), 1.0)
    c_prompt = nrm((BATCH, D), 1.0)
    c_sample = nrm((DEC_BATCH, D), 1.0)
    cache_ckv = nrm((L, n_phys, PAGE_SIZE, KV_LORA), 1.0)
    cache_krope = nrm((L, n_phys, PAGE_SIZE, QK_ROPE), 1.0)
    state_wkv = nrm((L, DEC_BATCH, RW_HEADS, RW_HEAD_DIM, RW_HEAD_DIM), 0.3)
    state_shift = nrm((L, DEC_BATCH, RW_COLS), 1.0)
    return {
        'x_prompt': x_prompt,
        'x_sample': x_sample,
        'c_prompt': c_prompt,
        'c_sample': c_sample,
        'cache_ckv': cache_ckv,
        'cache_krope': cache_krope,
        'state_wkv': state_wkv,
        'state_shift': state_shift,
        'page_table': page_table,
        'w_ada': nrm((L, D, 6 * D), D ** -0.5),
        'b_ada': nrm((L, 6 * D), 0.02),
        'w_in': nrm((L, D, IN_COLS), D ** -0.5) * col_scale,
        'mu_shift': jax.random.uniform(next(ks), (L, RW_COLS), f32),
        'w_decay0': -6.0 + 5.0 * jax.random.uniform(next(ks), (L, RW_WIDTH), f32),
        'w_decay_up': nrm((L, DECAY_LORA, RW_WIDTH), 0.1),
        'a0': nrm((L, RW_WIDTH), 0.1),
        'w_iclr_up': nrm((L, ICLR_LORA, RW_WIDTH), 0.5 * ICLR_LORA ** -0.5),
        'w_gate_up': nrm((L, GATE_LORA, RW_WIDTH), GATE_LORA ** -0.5),
        'k_k': 0.85 + nrm((L, RW_WIDTH), 0.02),
        'k_a': gain((L, RW_WIDTH)),
        'r_k': nrm((L, RW_WIDTH), 0.1),
        'lnx_g': gain((L, RW_WIDTH)),
        'lnx_b': nrm((L, RW_WIDTH), 0.02),
        'w_branch_a': nrm((L, RW_WIDTH, D), RW_WIDTH ** -0.5 * DN_BETA),
        'g_qnorm': gain((L, Q_LORA)),
        'w_uq': nrm((L, Q_LORA, MLA_HEADS * (QK_NOPE + QK_ROPE)), Q_LORA ** -0.5),
        'g_kvnorm': gain((L, KV_LORA)),
        'w_uk': nrm((L, KV_LORA, MLA_HEADS * QK_NOPE), KV_LORA ** -0.5),
        'w_uv': nrm((L, KV_LORA, MLA_HEADS * V_HEAD), KV_LORA ** -0.5 * DN_BETA),
        'w_branch_b': nrm((L, MLA_HEADS * V_HEAD, D), (MLA_HEADS * V_HEAD) ** -0.5 * DN_BETA),
        'w_out': nrm((L, D, D), D ** -0.5 * DN_BETA),
        'ln1_g': gain((L, D)),
        'ln1_b': nrm((L, D), 0.02),
        'w_router': nrm((L, D, N_EXPERTS), D ** -0.5),
        'b_router': nrm((L, N_EXPERTS), 0.01),
        'w_exp_gate': nrm((L, N_EXPERTS, D, EXPERT_FF), D ** -0.5 * DN_BETA),
        'w_exp_up': nrm((L, N_EXPERTS, D, EXPERT_FF), D ** -0.5 * DN_BETA),
        'w_exp_down': nrm((L, N_EXPERTS, EXPERT_FF, D), EXPERT_FF ** -0.5 * DN_BETA),
        'w_sh_gate': nrm((L, D, SHARED_FF), D ** -0.5 * DN_BETA),
        'w_sh_up': nrm((L, D, SHARED_FF), D ** -0.5 * DN_BETA),
        'w_sh_down': nrm((L, SHARED_FF, D), SHARED_FF ** -0.5 * DN_BETA),
        'ln2_g': gain((L, D)),
        'ln2_b': nrm((L, D), 0.02),
    }


def reference(x_prompt, x_sample, c_prompt, c_sample, cache_ckv, cache_krope, state_wkv, state_shift,
              page_table, w_ada, b_ada, w_in, mu_shift, w_decay0, w_decay_up, a0, w_iclr_up, w_gate_up,
              k_k, k_a, r_k, lnx_g, lnx_b, w_branch_a, g_qnorm, w_uq, g_kvnorm, w_uk, w_uv, w_branch_b,
              w_out, ln1_g, ln1_b, w_router, b_router, w_exp_gate, w_exp_up, w_exp_down,
              w_sh_gate, w_sh_up, w_sh_down, ln2_g, ln2_b):
    P = dict(w_ada=w_ada, b_ada=b_ada, w_in=w_in, mu_shift=mu_shift, w_decay0=w_decay0,
             w_decay_up=w_decay_up, a0=a0, w_iclr_up=w_iclr_up, w_gate_up=w_gate_up, k_k=k_k, k_a=k_a,
             r_k=r_k, lnx_g=lnx_g, lnx_b=lnx_b, w_branch_a=w_branch_a, g_qnorm=g_qnorm, w_uq=w_uq,
             g_kvnorm=g_kvnorm, w_uk=w_uk, w_uv=w_uv, w_branch_b=w_branch_b, w_out=w_out,
             ln1_g=ln1_g, ln1_b=ln1_b, w_router=w_router, b_router=b_router, w_exp_gate=w_exp_gate,
             w_exp_up=w_exp_up, w_exp_down=w_exp_down, w_sh_gate=w_sh_gate, w_sh_up=w_sh_up,
             w_sh_down=w_sh_down, ln2_g=ln2_g, ln2_b=ln2_b)
    bp, t_p = x_prompt.shape[0], x_prompt.shape[1]
    bd, s_new = x_sample.shape[0], x_sample.shape[1]
    past = page_table.shape[1] * PAGE_SIZE
    pos_p = jnp.arange(t_p, dtype=jnp.int32)
    pos_s = past + jnp.arange(s_new, dtype=jnp.int32)
    yp, ys = x_prompt, x_sample
    ckv_p, kr_p, wkv_p, sh_p = [], [], [], []
    ckv_s, kr_s, wkv_s, sh_s = [], [], [], []
    for l in range(DEPTH):
        Pl = {name: arr[l] for name, arr in P.items()}
        shift0 = jnp.zeros((bp, RW_COLS), x_prompt.dtype)
        wkv0 = jnp.zeros((bp, RW_HEADS, RW_HEAD_DIM, RW_HEAD_DIM), jnp.float32)
        yp, a_ckv, a_kr, a_wkv, a_sh = _layer(yp, c_prompt, shift0, wkv0, pos_p, _mla_prompt, Pl)
        past_ckv = cache_ckv[l][page_table].reshape(bd, past, KV_LORA)
        past_kr = cache_krope[l][page_table].reshape(bd, past, QK_ROPE)
        attend_s = functools.partial(_mla_sample, past_ckv, past_kr)
        ys, b_ckv, b_kr, b_wkv, b_sh = _layer(ys, c_sample, state_shift[l], state_wkv[l], pos_s, attend_s, Pl)
        ckv_p.append(a_ckv)
        kr_p.append(a_kr)
        wkv_p.append(a_wkv)
        sh_p.append(a_sh)
        ckv_s.append(b_ckv)
        kr_s.append(b_kr)
        wkv_s.append(b_wkv)
        sh_s.append(b_sh)
    return (yp, ys, jnp.stack(ckv_p), jnp.stack(kr_p), jnp.stack(wkv_p), jnp.stack(sh_p),
            jnp.stack(ckv_s), jnp.stack(kr_s), jnp.stack(wkv_s), jnp.stack(sh_s))
```

```python
import numpy as np
import concourse.bass as bass
import concourse.mybir as mybir

F32 = mybir.dt.float32
BF16 = mybir.dt.bfloat16
I32 = mybir.dt.int32
AF = mybir.ActivationFunctionType
ALU = mybir.AluOpType
AX = mybir.AxisListType

ENGS = ("pe", "act", "dve", "pool", "sp")
KDMA = 8


class Res:
    __slots__ = ("name", "w", "rc", "rd")

    def __init__(self, name):
        self.name = name
        self.w = None
        self.rc = {}
        self.rd = []


class Op:
    __slots__ = ("eng", "fn", "deps", "sig", "dma", "sem", "val")

    def __init__(self, eng, fn, dma):
        self.eng = eng
        self.fn = fn
        self.dma = dma
        self.deps = []
        self.sig = bool(dma)
        self.sem = None
        self.val = 0


class TL:
    __slots__ = ("t", "res")

    def __init__(self, t, name):
        self.t = t
        self.res = Res(name)

    def __getitem__(self, k):
        return self.t[k]


def _res(x):
    return x.res if isinstance(x, TL) else x


class Ctx:
    def __init__(self, nc):
        self.nc = nc
        self.q = {e: [] for e in ENGS}
        self.pending = {e: [] for e in ENGS}
        self.dma_since_bar = []
        self.nops = 0
        self.dead = False

    def op(self, eng, fn, reads=(), writes=(), dma=False):
        if self.dead:
            return None
        o = Op(eng, fn, dma)
        deps = {}
        for r in reads:
            r = _res(r)
            if r.w is not None:
                deps[id(r.w)] = r.w
        for r in writes:
            r = _res(r)
            if r.w is not None:
                deps[id(r.w)] = r.w
            for x in r.rc.values():
                deps[id(x)] = x
            for x in r.rd:
                deps[id(x)] = x
        for x in self.pending[eng]:
            deps[id(x)] = x
        self.pending[eng] = []
        dl = []
        for d in deps.values():
            if d is o:
                continue
            if d.eng == eng and (not d.dma) and (not dma) and eng == "pe":
                continue
            dl.append(d)
            d.sig = True
        o.deps = dl
        for r in reads:
            r = _res(r)
            if dma:
                r.rd.append(o)
            else:
                r.rc[eng] = o
        for r in writes:
            r = _res(r)
            r.w = o
            r.rc = {}
            r.rd = []
        self.q[eng].append(o)
        if dma:
            self.dma_since_bar.append(o)
        self.nops += 1
        return o

    def barrier(self):
        lasts = []
        for e in ENGS:
            for o in reversed(self.q[e]):
                if not o.dma:
                    lasts.append(o)
                    break
        lasts += self.dma_since_bar
        self.dma_since_bar = []
        for e in ENGS:
            self.pending[e] = self.pending[e] + list(lasts)

    def emit(self, stack):
        nc = self.nc
        csem = {}
        dsem = {}
        for e in ENGS:
            if e != "sp":
                csem[e] = stack.enter_context(nc.semaphore("c_" + e))
        for e in ("sp", "pool", "act"):
            dsem[e] = [stack.enter_context(nc.semaphore("d_%s%d" % (e, i))) for i in range(KDMA)]
        for e in ENGS:
            cnt = 0
            di = 0
            dcnt = [0] * KDMA
            dprev = [None] * KDMA
            for o in self.q[e]:
                if not o.sig:
                    continue
                if o.dma:
                    k = di % KDMA
                    di += 1
                    dcnt[k] += 16
                    o.sem = dsem[e][k]
                    o.val = dcnt[k]
                    if dprev[k] is not None and dprev[k] not in o.deps:
                        o.deps.append(dprev[k])
                    dprev[k] = o
                else:
                    cnt += 1
                    o.sem = csem[e]
                    o.val = cnt
        block = stack.enter_context(nc.Block())
        q = self.q

        def run(eh, e):
            seen = {}
            for o in q[e]:
                for d in o.deps:
                    key = d.sem.num
                    if seen.get(key, 0) < d.val:
                        eh.wait_ge(d.sem, d.val)
                        seen[key] = d.val
                inst = o.fn(eh)
                if o.sig:
                    inst.then_inc(o.sem, 16 if o.dma else 1)

        @block.tensor
        def _(eh):
            run(eh, "pe")

        @block.scalar
        def _(eh):
            run(eh, "act")

        @block.vector
        def _(eh):
            run(eh, "dve")

        @block.gpsimd
        def _(eh):
            run(eh, "pool")

        @block.sync
        def _(eh):
            run(eh, "sp")

import math
from contextlib import ExitStack
import numpy as np
import concourse.bass as bass
import concourse.mybir as mybir

D = 1024
DC = 8
RWC = 1792
NH = 8
LOGK = math.exp(-0.5)
GN_EPS = 64e-5
RMS_EPS = 1e-6
LN_EPS = 1e-5
ATTN_SCALE = 96 ** -0.5
ALPHA = 2 ** 0.25


class Cfg:
    def __init__(self, T=4096, PAST=8192, NPHYS=10240, NS=16, phases=("p0", "p1")):
        self.T = T
        self.NB = T // 128
        self.NBO = self.NB // 2
        self.PAST = PAST
        self.NPG = PAST // 128
        self.NPHYS = NPHYS
        self.NS = NS
        self.phases = phases
        self.debug = False
        self.stop_at = None
        self.NE = 65
        self.kvside = True
        self.sample = True
        self.debug_oa = True
        self.debug_ob = True


def host_consts(hf):
    c = {}
    c["ident"] = np.eye(128, dtype=np.float32)
    s = np.arange(128)[:, None]
    t = np.arange(128)[None, :]
    same = (s // 64) == (t // 64)
    tri = np.zeros((3, 128, 128), np.float32)
    tri[0] = np.where(same & (s <= t), -LOGK, 0.0)
    tri[1] = np.where(same & (s < t), -LOGK, 0.0)
    tri[2] = np.where(same, -LOGK, 0.0)
    c["tri"] = tri
    mus = (same & (s < t)).astype(np.float32)
    mui = (same & (s <= t)).astype(np.float32)
    mk1 = np.concatenate([mus, mui], 1)
    c["mk"] = np.concatenate([mk1, mk1], 1)
    ml1 = (same & (t < s)).astype(np.float32)
    c["ml"] = np.concatenate([ml1, ml1], 1)
    sh = np.zeros((2, 128, 128), np.float32)
    sh[0][np.arange(127), np.arange(1, 128)] = 1.0
    sh[1][127, 0] = 1.0
    c["shm"] = sh
    c["selc"] = np.tile(np.array([[1.0 - hf, float(hf)]], np.float32), (128, 1))
    return c


def build(cfg):
    nc = bass.Bass("TRN2", target_bir_lowering=False)
    T, NB = cfg.T, cfg.NB
    stack = ExitStack()
    ctx = Ctx(nc)

    def din(name, shape, dt=F32):
        return nc.dram_tensor(name, list(shape), dt, kind="ExternalInput").ap()

    def dout(name, shape, dt=F32):
        return nc.dram_tensor(name, list(shape), dt, kind="ExternalOutput").ap()

    def sb(name, shape, dt=F32):
        return TL(stack.enter_context(nc.sbuf_tensor(name, list(shape), dt)), name)

    xp = din("xp", [T, D])
    cv = din("cv", [17, D])
    w_ada = din("w_ada", [D, 6 * D])
    b_ada = din("b_ada", [48, 128])
    w_rw = din("w_rw", [D, RWC])
    mu = din("mu", [1, RWC])
    pvec = din("pvec", [1, 7 * 512])
    w_du = din("w_du", [64, 512])
    w_iu = din("w_iu", [64, 512])
    w_gu = din("w_gu", [128, 512])
    c_ident = din("ident", [128, 128])
    c_tri = din("tri", [3, 128, 128])
    c_mk = din("mk", [128, 512])
    c_ml = din("ml", [128, 256])
    c_shm = din("shm", [2, 128, 128])
    c_selc = din("selc", [128, 2])

    w_kv = din("w_kv", [D, 320])
    gkv = din("gkv", [1, 256])
    ropep = din("ropep", [T, 64])
    ropes = din("ropes", [1, 64])
    xs = din("xs", [16, D])
    st_shift = din("st_shift", [16, RWC])
    o_ckv = dout("o_ckv", [T, 256])
    o_kr = dout("o_kr", [T, 32])
    o_shift_s = dout("o_shift_s", [16, RWC])
    o_smp = dout("o_smp", [16, 320])
    st_wkv = din("st_wkv", [128, 4096])
    o_wkv_s = dout("o_wkv_s", [128, 4096])
    o_wkv = dout("o_wkv", [8, 64, 64])
    o_shift = dout("o_shift", [1, RWC])

    PS = [TL(stack.enter_context(nc.psum_tensor("ps%d" % i, [128, 512], F32)), "ps%d" % i) for i in range(8)]

    def psb(i):
        return PS[i].t[:, :].bitcast(BF16)

    cnt = [0]

    def ld(eng, out_tl, out_ap, in_ap, extra_r=()):
        return ctx.op(eng, lambda e: e.dma_start(out=out_ap, in_=in_ap), reads=extra_r, writes=[out_tl], dma=True)

    def st(eng, out_ap, in_tl, in_ap, dres=None):
        return ctx.op(eng, lambda e: e.dma_start(out=out_ap, in_=in_ap), reads=[in_tl], writes=[dres] if dres else [], dma=True)

    pe_state = {"seq": 0, "full": -1, "bank": {}}

    def pe_guard(lhsT, out_tl):
        if ctx.dead:
            return
        p0 = lhsT.base_partition()
        p1 = p0 + lhsT.partition_size()
        st_ = pe_state
        lb = st_["bank"].get(id(out_tl))
        if lb is not None and (p1 <= lb[0] or lb[1] <= p0) and st_["full"] < lb[2]:
            st_["seq"] += 1
            st_["full"] = st_["seq"]
            ctx.op("pe", lambda e: e.matmul(PS[7][:, 0:1], identb[:, :], identb[:, 0:1], start=True, stop=True), reads=[identb], writes=[PS[7]])
        st_["seq"] += 1
        if p0 == 0 and p1 == 128:
            st_["full"] = st_["seq"]
        st_["bank"][id(out_tl)] = (p0, p1, st_["seq"])

    def mm(out_tl, out_ap, lhsT, rhs, start, stop, reads):
        pe_guard(lhsT, out_tl)
        return ctx.op("pe", lambda e: e.matmul(out_ap, lhsT, rhs, start=start, stop=stop), reads=reads, writes=[out_tl])

    def tr(out_tl, out_ap, in_ap, ident_ap, reads):
        pe_guard(in_ap, out_tl)
        return ctx.op("pe", lambda e: e.transpose(out_ap, in_ap, ident_ap), reads=reads, writes=[out_tl])

    def act(out_tl, out_ap, in_ap, func, reads, bias=None, scale=None, eng="act"):
        kw = {}
        if bias is not None:
            kw["bias"] = bias
        if scale is not None:
            kw["scale"] = scale
        return ctx.op(eng, lambda e: e.activation(out=out_ap, in_=in_ap, func=func, **kw), reads=reads, writes=[out_tl])

    def tt(eng, out_tl, out_ap, in0, in1, op, reads):
        return ctx.op(eng, lambda e: e.tensor_tensor(out=out_ap, in0=in0, in1=in1, op=op), reads=reads, writes=[out_tl])

    def ts(eng, out_tl, out_ap, in0, s1, s2, op0, op1, reads):
        if op1 is None:
            return ctx.op(eng, lambda e: e.tensor_scalar(out=out_ap, in0=in0, scalar1=s1, scalar2=None, op0=op0), reads=reads, writes=[out_tl])
        return ctx.op(eng, lambda e: e.tensor_scalar(out=out_ap, in0=in0, scalar1=s1, scalar2=s2, op0=op0, op1=op1), reads=reads, writes=[out_tl])

    def stt(eng, out_tl, out_ap, in0, scalar, in1, op0, op1, reads):
        return ctx.op(eng, lambda e: e.scalar_tensor_tensor(out=out_ap, in0=in0, scalar=scalar, in1=in1, op0=op0, op1=op1), reads=reads, writes=[out_tl])

    def cp(eng, out_tl, out_ap, in_ap, reads):
        if eng == "act":
            return ctx.op(eng, lambda e: e.copy(out=out_ap, in_=in_ap), reads=reads, writes=[out_tl])
        return ctx.op(eng, lambda e: e.tensor_copy(out=out_ap, in_=in_ap), reads=reads, writes=[out_tl])

    def red(eng, out_tl, out_ap, in_ap, op, reads):
        return ctx.op(eng, lambda e: e.tensor_reduce(out=out_ap, in_=in_ap, axis=AX.X, op=op), reads=reads, writes=[out_tl])

    def recip(tl, out_ap, in_ap, reads):
        return ctx.op("dve", lambda e: e.reciprocal(out=out_ap, in_=in_ap), reads=reads, writes=[tl])

    def mset(eng, tl, ap, val):
        return ctx.op(eng, lambda e: e.memset(ap, val), reads=[], writes=[tl])

    DBG = {}

    def mark(n):
        if cfg.stop_at is not None and cfg.stop_at == n:
            ctx.dead = True

    ASTK = [stack]

    def dbg(name, tl, ap):
        if not cfg.debug:
            return
        shp = list(ap.shape)
        if ap.dtype != F32:
            tmp = TL(ASTK[-1].enter_context(nc.sbuf_tensor("dbgt_" + name, shp, F32)), "dbgt_" + name)
            cp("dve", tmp, tmp.t[tuple(slice(None) for _ in shp)], ap, [tl])
            tl, ap = tmp, tmp.t[tuple(slice(None) for _ in shp)]
        d = dout("dbg_" + name, shp)
        st("sp", d[tuple(slice(None) for _ in shp)], tl, ap)
        DBG[name] = shp

    ident = sb("identf", [128, 128])
    identb = sb("identb", [128, 128], BF16)
    ones1 = sb("ones1", [128, 1])
    selc = sb("selc_sb", [128, 2])
    ld("sp", ident, ident[:, :], c_ident[:, :])
    ld("pool", identb, identb[:, :], c_ident[:, :])
    ld("sp", selc, selc[:, :], c_selc[:, :])
    mset("dve", ones1, ones1[:, :], 1.0)

    MODT = sb("modt", [128, 48, 17])
    modr_d = nc.dram_tensor("modr_d", [1, 6144], F32, kind="Internal").ap()
    modr_res = Res("modr_d")
    modrs_d = nc.dram_tensor("modrs_d", [16, 6144], F32, kind="Internal").ap()
    modrs_res = Res("modrs_d")
    oas_d = nc.dram_tensor("oas_d", [16, 512], F32, kind="Internal").ap()
    oas_res = Res("oas_d")
    obs_d = nc.dram_tensor("obs_d", [16, 512], F32, kind="Internal").ap()
    obs_res = Res("obs_d")
    oa_d = nc.dram_tensor("oa_d", [cfg.NBO * 128, 512], F32, kind="Internal").ap()
    oa_res = Res("oa_d")

    if "p0" in cfg.phases:
        with ExitStack() as s0:
            def sb0(name, shape, dt=F32):
                return TL(s0.enter_context(nc.sbuf_tensor(name, list(shape), dt)), name)
            cvt = sb0("cvt", [17, D])
            scT = sb0("scT", [128, 8, 17], BF16)
            bat = sb0("bat", [48, 128])
            baT = sb0("baT", [128, 48])
            wa = [sb0("wa%d" % i, [128, 8, 512], BF16) for i in range(2)]
            ld("sp", cvt, cvt[:, :], cv[:, :])
            ld("sp", bat, bat[:, :], b_ada[:, :])
            for kc in range(8):
                tr(PS[0], PS[0][:, kc * 17:(kc + 1) * 17], cvt[:, kc * 128:(kc + 1) * 128], ident[0:17, 0:17], [cvt, ident])
            act(scT, scT[:, :, :], PS[0][:, 0:136].rearrange("p (a b) -> p a b", a=8), AF.Silu, [PS[0]])
            tr(PS[1], PS[1][:, 0:48], bat[:, :], ident[0:48, 0:48], [bat, ident])
            cp("dve", baT, baT[:, :], PS[1][:, 0:48], [PS[1]])
            w_ada_v = w_ada.rearrange("(kc p) n -> p kc n", p=128)
            for grp in range(12):
                w = wa[grp % 2]
                ld("pool", w, w[:, :, :], w_ada_v[:, :, grp * 512:(grp + 1) * 512])
                pb = PS[2 + grp % 2]
                for cc in range(4):
                    col = grp * 4 + cc
                    for kc in range(8):
                        mm(pb, pb[:, cc * 17:(cc + 1) * 17], w[:, kc, cc * 128:(cc + 1) * 128], scT[:, kc, :],
                           kc == 0, kc == 7, [w, scT])
                for cc in range(4):
                    col = grp * 4 + cc
                    act(MODT, MODT[:, col, :], pb[:, cc * 17:(cc + 1) * 17], AF.Identity, [pb, baT],
                        bias=baT[:, col:col + 1], scale=1.0)
            bar = sb0("bar", [1, 6144])
            MODR = sb0("modr", [1, 6144])
            MODRS = sb0("modrs", [16, 6144])
            bars = sb0("bars", [16, 6144])
            ld("sp", bars, bars[:, :], b_ada.rearrange("a b -> (a b)")[None, :].partition_broadcast(16))
            ld("sp", bar, bar[:, :], b_ada.rearrange("a b -> (a b)")[None, :])
            for grp in range(12):
                w = wa[grp % 2]
                ld("pool", w, w[:, :, :], w_ada_v[:, :, grp * 512:(grp + 1) * 512])
                pb = PS[4 + grp % 2]
                for kc in range(8):
                    mm(pb, pb[0:1, :], scT[:, kc, 0:1], w[:, kc, :], kc == 0, kc == 7, [w, scT])
                tt("dve", MODR, MODR[0:1, grp * 512:(grp + 1) * 512], pb[0:1, :], bar[0:1, grp * 512:(grp + 1) * 512], ALU.add, [pb, bar])
                pb2 = PS[6 + grp % 2]
                for kc in range(8):
                    mm(pb2, pb2[0:16, :], scT[:, kc, 1:17], w[:, kc, :], kc == 0, kc == 7, [w, scT])
                tt("dve", MODRS, MODRS[0:16, grp * 512:(grp + 1) * 512], pb2[0:16, :], bars[0:16, grp * 512:(grp + 1) * 512], ALU.add, [pb2, bars])
            st("sp", modr_d[:, :], MODR, MODR[:, :], dres=modr_res)
            st("sp", modrs_d[:, :], MODRS, MODRS[:, :], dres=modrs_res)
            ts("dve", MODT, MODT[:, 8:16, :], MODT[:, 8:16, :], 1.0, None, ALU.add, None, [MODT])
            ts("dve", MODT, MODT[:, 32:40, :], MODT[:, 32:40, :], 1.0, None, ALU.add, None, [MODT])
            ctx.barrier()

    sM = None
    if "p1" in cfg.phases:
        sM = ExitStack()

        def sbm(name, shape, dt=F32):
            return TL(sM.enter_context(nc.sbuf_tensor(name, list(shape), dt)), name)
        Wrw = sbm("wrw", [128, 8, RWC], BF16)
        Wlo = sbm("wlo", [128, 512], BF16)
        Wgu = sbm("wgu", [128, 512], BF16)
        PV = sbm("pv", [128, 7, 512])
        MUb = sbm("mub", [128, RWC])
        Wkv = sbm("wkv_sb", [128, 8, 320], BF16)
        GKV = sbm("gkv_sb", [128, 256])
        RPs = sbm("rps", [16, 64])
        with ExitStack() as s1:
            ASTK.append(s1)
            def sb1(name, shape, dt=F32):
                return TL(s1.enter_context(nc.sbuf_tensor(name, list(shape), dt)), name)
            TRI = sb1("tri_sb", [128, 3, 128])
            MK = sb1("mk_sb", [128, 512])
            ML = sb1("ml_sb", [128, 256])
            SHM = sb1("shm_sb", [128, 2, 128])
            ld("pool", Wrw, Wrw[:, :, :], w_rw.rearrange("(kc p) n -> p kc n", p=128))
            ld("pool", Wlo, Wlo[0:64, :], w_du[:, :])
            ld("pool", Wlo, Wlo[64:128, :], w_iu[:, :])
            ld("pool", Wgu, Wgu[:, :], w_gu[:, :])
            ld("sp", PV, PV[:, :, :].rearrange("p a b -> p (a b)"), pvec.partition_broadcast(128))
            ld("sp", MUb, MUb[:, :], mu.partition_broadcast(128))
            ld("sp", TRI, TRI[:, :, :], c_tri.rearrange("a p n -> p a n"))
            ld("sp", MK, MK[:, :], c_mk[:, :])
            ld("sp", ML, ML[:, :], c_ml[:, :])
            ld("sp", SHM, SHM[:, :, :], c_shm.rearrange("a p n -> p a n"))
            P_W0, P_A0, P_KK, P_KA, P_RK, P_LG, P_LB = range(7)
            ld("pool", Wkv, Wkv[:, :, :], w_kv.rearrange("(kc p) n -> p kc n", p=128))
            ld("sp", GKV, GKV[:, :], gkv.partition_broadcast(128))
            ld("sp", RPs, RPs[:, :], ropes.partition_broadcast(16))
            rpt = [sb1("rpt%d" % i, [128, 64]) for i in range(2)]
            kvs = sb1("kvs", [128, 2])
            ckvt = [sb1("ckvt%d" % i, [128, 256]) for i in range(2)]
            krt = [sb1("krt%d" % i, [128, 32]) for i in range(2)]
            krtmp = sb1("krtmp", [128, 32])

            def kv_side(ntok, hT_ap_fn, h_tl, ps_tl, rope_tl, rope_ap, ck_tl, kr_tl, tmp_sq):
                for kc in range(8):
                    mm(ps_tl, ps_tl[0:ntok, 0:320], hT_ap_fn(kc), Wkv[:, kc, :], kc == 0, kc == 7, [h_tl, Wkv])
                act(tmp_sq, tmp_sq[0:ntok, 0:256], ps_tl[0:ntok, 0:256], AF.Square, [ps_tl])
                red("dve", kvs, kvs[0:ntok, 0:1], tmp_sq[0:ntok, 0:256], ALU.add, [tmp_sq])
                ts("dve", kvs, kvs[0:ntok, 1:2], kvs[0:ntok, 0:1], 1.0 / 256, RMS_EPS, ALU.mult, ALU.add, [kvs])
                act(kvs, kvs[0:ntok, 1:2], kvs[0:ntok, 1:2], AF.Sqrt, [kvs])
                ctx.op("dve", lambda e: e.reciprocal(out=kvs[0:ntok, 1:2], in_=kvs[0:ntok, 1:2]), reads=[kvs], writes=[kvs])
                stt("dve", ck_tl, ck_tl[0:ntok, :], ps_tl[0:ntok, 0:256], kvs[0:ntok, 1:2], GKV[0:ntok, :], ALU.mult, ALU.mult, [ps_tl, kvs, GKV])
                tt("dve", kr_tl, kr_tl[0:ntok, :], ps_tl[0:ntok, 256:288], rope_ap[:, 0:32], ALU.mult, [ps_tl, rope_tl])
                tt("dve", krtmp, krtmp[0:ntok, :], ps_tl[0:ntok, 288:320], rope_ap[:, 32:64], ALU.mult, [ps_tl, rope_tl])
                tt("dve", kr_tl, kr_tl[0:ntok, :], kr_tl[0:ntok, :], krtmp[0:ntok, :], ALU.add, [kr_tl, krtmp])

            xt = [sb1("xt%d" % i, [128, D]) for i in range(2)]
            hT = [sb1("hT%d" % i, [128, 8, 128], BF16) for i in range(2)]
            prw = [sb1("prw%d" % i, [128, RWC]) for i in range(2)]
            pm = sb1("pm", [128, RWC])
            lo_tm = sb1("lo_tm", [128, 256], BF16)
            loT = sb1("loT", [128, 2, 128], BF16)
            tA = sb1("tA", [128, 512])
            tB = sb1("tB", [128, 512])
            tC = sb1("tC", [128, 512])
            t1 = t2 = Lsb = otmp = tA
            kk0 = dLC = osq = tB
            sq = t3 = rk = on = tC
            sg = sb1("sg", [128, 512])
            aa = sb1("aa", [128, 512])
            gsb = sb1("gsb", [128, 512])
            ss = sb1("ss", [128, 8])
            rn = sb1("rn", [128, 8])
            kkn = sb1("kkn", [128, 512])
            kmod = sb1("kmod", [128, 512])
            bb = sb1("bb", [128, 512])
            bs = sb1("bs", [128, 8])
            bonus = sb1("bonus", [128, 512])
            eL = sb1("eL", [128, 512])
            eLm = sb1("eLm", [128, 512])
            enL = sb1("enL", [128, 512])
            eLcL = sb1("eLcL", [128, 512])
            eLC = sb1("eLC", [128, 512])
            FMs = sb1("FMs", [128, 4, 512], BF16)
            Kp = sb1("Kp", [128, 512], BF16)
            Bpn = sb1("Bpn", [128, 512], BF16)
            Vbf = sb1("Vbf", [128, 512], BF16)
            FM = sb1("FM", [128, 4, 4, 128], BF16)
            PCt = sb1("PCt", [128, 4, 2])
            MN = [sb1("MN%d" % g, [128, 2, 256], BF16) for g in range(4)]
            NBR = [sb1("NBR%d" % g, [128, 2, 128], BF16) for g in range(4)]
            APX = [[sb1("APX%d_%d" % (q, i), [128, 4, 256]) for i in range(2)] for q in range(2)]
            NPX = [[sb1("NPX%d_%d" % (q, i), [128, 4, 128]) for i in range(2)] for q in range(2)]
            TTb = [sb1("TTb%d" % q, [128, 4, 128], BF16) for q in range(2)]
            Sf = sb1("Sf", [128, 4, 64])
            Sdec = sb1("Sdec", [128, 4, 64])
            Sbf = [sb1("Sbf%d" % i, [128, 4, 64], BF16) for i in range(2)]
            Wsb = sb1("Wsb", [128, 512], BF16)
            Ubf = sb1("Ubf", [128, 512], BF16)
            osb = sb1("osb", [128, 512])
            s1t = sb1("s1t", [128, 8])
            s2t = sb1("s2t", [128, 8])
            mean = sb1("mean", [128, 8])
            var = sb1("var", [128, 8])
            msq = sb1("msq", [128, 8])
            rstd = sb1("rstd", [128, 8])
            ofin = sb1("ofin", [128, 512], BF16)
            oev = sb1("oev", [128, 512], BF16)
            oab = [sb1("oab%d" % i, [128, 512]) for i in range(2)]

            mset("dve", prw[1], prw[1][:, :], 0.0)
            mset("dve", Sf, Sf[:, :, :], 0.0)
            mset("dve", Sbf[0], Sbf[0][:, :, :], 0.0)
            scur = 0

            def v3(ap):
                return ap.rearrange("p (h d) -> p h d", h=8)

            for blk in range(NB):
                cur = blk % 2
                prv = 1 - cur
                x_ = xt[cur]
                ld("sp", x_, x_[:, :], xp[blk * 128:(blk + 1) * 128, :])
                for kc in range(8):
                    pb = PS[kc // 4]
                    tr(pb, pb[:, (kc % 4) * 128:(kc % 4 + 1) * 128], x_[:, kc * 128:(kc + 1) * 128], ident[:, :], [x_, ident])
                mark(1)
                h_ = hT[cur]
                for kc in range(8):
                    pb = PS[kc // 4]
                    act(h_, h_[:, kc, :], pb[:, (kc % 4) * 128:(kc % 4 + 1) * 128], AF.Identity, [pb, MODT],
                        bias=MODT[:, kc, 0:1], scale=MODT[:, 8 + kc, 0:1])
                if cfg.kvside:
                    rp_ = rpt[cur]
                    ld("sp", rp_, rp_[:, :], ropep[blk * 128:(blk + 1) * 128, :])
                    kv_side(128, lambda kc: h_[:, kc, :], h_, PS[6], rp_, rp_[:, :], ckvt[cur], krt[cur], tC)
                    st("sp", o_ckv[blk * 128:(blk + 1) * 128, :], ckvt[cur], ckvt[cur][:, :])
                    st("sp", o_kr[blk * 128:(blk + 1) * 128, :], krt[cur], krt[cur][:, :])
                mark(2)
                ngr = [(0, 512), (512, 512), (1024, 512), (1536, 256)]
                for n, (c0, cw) in enumerate(ngr):
                    pb = PS[2 + n]
                    for kc in range(8):
                        mm(pb, pb[:, 0:cw], h_[:, kc, :], Wrw[:, kc, c0:c0 + cw], kc == 0, kc == 7, [h_, Wrw])
                p_ = prw[cur]
                for n, (c0, cw) in enumerate(ngr):
                    pb = PS[2 + n]
                    if n % 2 == 0:
                        cp("act", p_, p_[:, c0:c0 + cw], pb[:, 0:cw], [pb])
                    else:
                        cp("dve", p_, p_[:, c0:c0 + cw], pb[:, 0:cw], [pb])
                mark(3)
                for n, (c0, cw) in enumerate(ngr):
                    pb = PS[2 + n]
                    mm(pb, pb[:, 0:cw], SHM[:, 0, :], p_[:, c0:c0 + cw], True, False, [SHM, p_])
                    mm(pb, pb[:, 0:cw], SHM[:, 1, :], prw[prv][:, c0:c0 + cw], False, True, [SHM, prw[prv]])
                for n, (c0, cw) in enumerate(ngr):
                    pb = PS[2 + n]
                    tt("dve", pm, pm[:, c0:c0 + cw], pb[:, 0:cw], p_[:, c0:c0 + cw], ALU.subtract, [pb, p_])
                tt("pool", pm, pm[:, :], pm[:, :], MUb[:, :], ALU.mult, [pm, MUb])
                tt("pool", pm, pm[:, :], pm[:, :], p_[:, :], ALU.add, [pm, p_])
                mark(4)
                act(lo_tm, lo_tm[:, 0:64], pm[:, 1536:1600], AF.Tanh, [pm])
                cp("dve", lo_tm, lo_tm[:, 64:128], pm[:, 1600:1664], [pm])
                act(lo_tm, lo_tm[:, 128:256], pm[:, 1664:1792], AF.Sigmoid, [pm])
                for i in range(2):
                    tr(PS[0], psb(0)[:, i * 128:(i + 1) * 128], lo_tm[:, i * 128:(i + 1) * 128], identb[:, :], [lo_tm, identb])
                cp("dve", loT, loT[:, :, :], psb(0)[:, 0:256].rearrange("p (a b) -> p a b", a=2), [PS[0]])
                mm(PS[1], PS[1][:, :], loT[0:64, 0, :], Wlo[0:64, :], True, True, [loT, Wlo])
                mm(PS[6], PS[6][:, :], loT[64:128, 0, :], Wlo[64:128, :], True, True, [loT, Wlo])
                mm(PS[5], PS[5][:, :], loT[:, 1, :], Wgu[:, :], True, True, [loT, Wgu])
                mark(5)
                r_ap = pm[:, 0:512]
                k_ap = pm[:, 512:1024]
                v_ap = pm[:, 1024:1536]
                tt("dve", t1, t1[:, :], PS[1][:, :], PV[:, P_W0, :], ALU.add, [PS[1], PV])
                act(sg, sg[:, :], t1[:, :], AF.Sigmoid, [t1])
                tt("dve", t2, t2[:, :], PS[6][:, :], PV[:, P_A0, :], ALU.add, [PS[6], PV])
                act(aa, aa[:, :], t2[:, :], AF.Sigmoid, [t2])
                cp("act", gsb, gsb[:, :], PS[5][:, :], [PS[5]])
                tt("pool", kk0, kk0[:, :], k_ap, PV[:, P_KK, :], ALU.mult, [pm, PV])
                tt("pool", sq, sq[:, :], kk0[:, :], kk0[:, :], ALU.mult, [kk0])
                red("dve", ss, ss[:, :], v3(sq[:, :]), ALU.add, [sq])
                ts("dve", rn, rn[:, :], ss[:, :], 1e-12, None, ALU.add, None, [ss])
                act(rn, rn[:, :], rn[:, :], AF.Sqrt, [rn])
                ctx.op("dve", lambda e: e.reciprocal(out=rn[:, :], in_=rn[:, :]), reads=[rn], writes=[rn])
                tt("dve", kkn, v3(kkn[:, :]), v3(kk0[:, :]), rn[:, :, None].broadcast_to([128, 8, 64]), ALU.mult, [kk0, rn])
                stt("dve", t3, t3[:, :], aa[:, :], -1.0, PV[:, P_KA, :], ALU.add, ALU.mult, [aa, PV])
                stt("dve", kmod, kmod[:, :], t3[:, :], 1.0, k_ap, ALU.add, ALU.mult, [t3, pm])
                tt("pool", bb, bb[:, :], kkn[:, :], aa[:, :], ALU.mult, [kkn, aa])
                tt("pool", rk, rk[:, :], r_ap, kmod[:, :], ALU.mult, [pm, kmod])
                tt("pool", rk, rk[:, :], rk[:, :], PV[:, P_RK, :], ALU.mult, [rk, PV])
                red("dve", bs, bs[:, :], v3(rk[:, :]), ALU.add, [rk])
                tt("pool", bonus, v3(bonus[:, :]), v3(v_ap), bs[:, :, None].broadcast_to([128, 8, 64]), ALU.mult, [pm, bs])
                mark(6)
                mm(PS[2], PS[2][:, :], TRI[:, 0, :], sg[:, :], True, True, [TRI, sg])
                mm(PS[3], PS[3][:, :], TRI[:, 1, :], sg[:, :], True, True, [TRI, sg])
                mm(PS[4], PS[4][:, :], TRI[:, 2, :], sg[:, :], True, True, [TRI, sg])
                act(eL, eL[:, :], PS[2][:, :], AF.Exp, [PS[2]])
                act(enL, enL[:, :], PS[2][:, :], AF.Exp, [PS[2]], scale=-1.0)
                cp("dve", Lsb, Lsb[:, :], PS[2][:, :], [PS[2]])
                act(eLm, eLm[:, :], PS[3][:, :], AF.Exp, [PS[3]])
                tt("dve", dLC, dLC[:, :], PS[4][:, :], Lsb[:, :], ALU.subtract, [PS[4], Lsb])
                act(eLcL, eLcL[:, :], dLC[:, :], AF.Exp, [dLC])
                act(eLC, eLC[:, :], PS[4][:, :], AF.Exp, [PS[4]])
                tt("pool", FMs, FMs[:, 0, :], kkn[:, :], eLm[:, :], ALU.mult, [kkn, eLm])
                tt("pool", FMs, FMs[:, 1, :], r_ap, eL[:, :], ALU.mult, [pm, eL])
                tt("dve", FMs, FMs[:, 2, :], kmod[:, :], enL[:, :], ALU.mult, [kmod, enL])
                tt("dve", FMs, FMs[:, 3, :], bb[:, :], enL[:, :], ALU.mult, [bb, enL])
                tt("pool", Kp, Kp[:, :], kmod[:, :], eLcL[:, :], ALU.mult, [kmod, eLcL])
                stt("dve", Bpn, Bpn[:, :], bb[:, :], -1.0, eLcL[:, :], ALU.mult, ALU.mult, [bb, eLcL])
                cp("act", Vbf, Vbf[:, :], v_ap, [pm])
                mark(7)
                for a in range(4):
                    for g in range(4):
                        idx = a * 4 + g
                        pbi = 5 + idx // 8
                        tr(PS[pbi], psb(pbi)[:, (idx % 8) * 128:(idx % 8 + 1) * 128], FMs[:, a, g * 128:(g + 1) * 128], identb[:, :], [FMs, identb])
                for a in range(4):
                    pbi = 5 + a // 2
                    src = psb(pbi)[:, (a % 2) * 512:(a % 2 + 1) * 512].rearrange("p (g t) -> p g t", g=4)
                    cp("act" if a % 2 == 0 else "dve", FM, FM[:, :, a, :], src, [PS[pbi]])
                mark(8)
                for g in range(4):
                    for c in range(2):
                        mm(PS[4], PS[4][:, g * 2 + c:g * 2 + c + 1], eLC[64 * c:64 * c + 1, g * 128:(g + 1) * 128], ones1[64 * c:64 * c + 1, 0:1], True, True, [eLC, ones1])
                cp("dve", PCt, PCt[:, :, :], PS[4][:, 0:8].rearrange("p (g c) -> p g c", g=4), [PS[4]])
                mark(9)
                for g in range(4):
                    q = g // 2
                    p1 = PS[0 + (g % 2) * 3]
                    p2 = PS[1 + (g % 2) * 3]
                    p3 = PS[2 + (g % 2) * 3]
                    for j in range(2):
                        ph = 64 * j
                        kt_rt = FM[ph:ph + 64, g, 0:2, :].rearrange("p a t -> p (a t)")
                        mm(p1, p1[:, j * 256:(j + 1) * 256], FM[ph:ph + 64, g, 2, :], kt_rt, True, True, [FM])
                        mm(p2, p2[:, j * 256:(j + 1) * 256], FM[ph:ph + 64, g, 3, :], kt_rt, True, True, [FM])
                        mm(p3, p3[:, j * 128:(j + 1) * 128], FM[ph:ph + 64, g, 0, :], FM[ph:ph + 64, g, 3, :], True, True, [FM])
                    import os
                    VAR = os.environ.get("VAR", "")
                    if VAR == "A":
                        continue
                    tt("dve", MN[g], MN[g][:, :, :].rearrange("p j c -> p (j c)"), p1[:, :], MK[:, :], ALU.mult, [p1, MK])
                    if VAR == "B":
                        continue
                    ax = APX[q][0]
                    hh0 = 2 * (g % 2)
                    p2v = p2[:, :].rearrange("p (j c) -> p j c", j=2)
                    mkv = MK[:, :].rearrange("p (j c) -> p j c", j=2)
                    stt("dve", ax, ax[:, hh0:hh0 + 2, 0:128], p2v[:, :, 0:128], -1.0, mkv[:, :, 0:128], ALU.mult, ALU.mult, [p2, MK])
                    if VAR == "C":
                        continue
                    stt("dve", NBR[g], NBR[g][:, :, :], p2v[:, :, 128:256], -1.0, mkv[:, :, 128:256], ALU.mult, ALU.mult, [p2, MK])
                    if VAR == "D":
                        continue
                    nx = NPX[q][0]
                    stt("dve", nx, nx[:, hh0:hh0 + 2, :], p3[:, 0:256].rearrange("p (j c) -> p j c", j=2), -1.0,
                        ML[:, :].rearrange("p (j c) -> p j c", j=2), ALU.mult, ALU.mult, [p3, ML])
                mark(10)
                for q in range(2):
                    ax = APX[q][0]
                    cp("pool", ax, ax[:, :, 128:256], ident[:, None, :].broadcast_to([128, 4, 128]), [ident])
                    pa0, pa1, pn = PS[3 * q], PS[3 * q + 1], PS[3 * q + 2]
                    for lvl in range(6):
                        a_c, a_n = APX[q][lvl % 2], APX[q][1 - lvl % 2]
                        n_c, n_n = NPX[q][lvl % 2], NPX[q][1 - lvl % 2]
                        for hh in range(4):
                            pa = pa0 if hh < 2 else pa1
                            mm(pa, pa[:, (hh % 2) * 256:(hh % 2 + 1) * 256], n_c[:, hh, :], a_c[:, hh, :], True, True, [n_c, a_c])
                            if lvl < 5:
                                mm(pn, pn[:, hh * 128:(hh + 1) * 128], a_c[:, hh, 0:128], n_c[:, hh, :], True, True, [a_c, n_c])
                        for half, pa in enumerate((pa0, pa1)):
                            pav = pa[:, :].rearrange("p (j c) -> p j c", j=2)
                            if lvl < 5:
                                cp("act", a_n, a_n[:, 2 * half:2 * half + 2, 0:128], pav[:, :, 0:128], [pa])
                            if lvl < 5:
                                tt("dve", a_n, a_n[:, 2 * half:2 * half + 2, 128:256], pav[:, :, 128:256], a_c[:, 2 * half:2 * half + 2, 128:256], ALU.add, [pa, a_c])
                            else:
                                tt("dve", TTb[q], TTb[q][:, 2 * half:2 * half + 2, :], pav[:, :, 128:256], a_c[:, 2 * half:2 * half + 2, 128:256], ALU.add, [pa, a_c])
                        if lvl < 5:
                            cp("act", n_n, n_n[:, :, :].rearrange("p h c -> p (h c)"), pn[:, :], [pn])
                mark(11)
                for c in range(2):
                    rc = slice(64 * c, 64 * c + 64)
                    tcs = slice(64 * c, 64 * c + 64)
                    S_c = Sbf[scur]
                    S_n = Sbf[1 - scur]
                    for h in range(8):
                        g, j = h // 2, h % 2
                        ph = slice(64 * j, 64 * j + 64)
                        hc = slice(h * 64, h * 64 + 64)
                        mm(PS[6], PS[6][rc, hc], FM[ph, g, 0, tcs], S_c[ph, g, :], True, False, [FM, S_c])
                        mm(PS[6], PS[6][rc, hc], MN[g][rc, j, 64 * c:64 * c + 64], Vbf[rc, hc], False, True, [MN[g], Vbf])
                    cp("act", Wsb, Wsb[rc, :], PS[6][rc, :], [PS[6]])
                    for h in range(8):
                        q, hh = h // 4, h % 4
                        hc = slice(h * 64, h * 64 + 64)
                        TTt = TTb[q]
                        mm(PS[3], PS[3][rc, hc], TTt[rc, hh, 64 * c:64 * c + 64], Wsb[rc, hc], True, True, [TTt, Wsb])
                    cp("dve", Ubf, Ubf[rc, :], PS[3][rc, :], [PS[3]])
                    for h in range(8):
                        g, j = h // 2, h % 2
                        ph = slice(64 * j, 64 * j + 64)
                        hc = slice(h * 64, h * 64 + 64)
                        mm(PS[5], PS[5][rc, hc], FM[ph, g, 1, tcs], S_c[ph, g, :], True, False, [FM, S_c])
                        mm(PS[5], PS[5][rc, hc], MN[g][rc, j, 128 + 64 * c:128 + 64 * c + 64], Vbf[rc, hc], False, False, [MN[g], Vbf])
                        mm(PS[5], PS[5][rc, hc], NBR[g][rc, j, 64 * c:64 * c + 64], Ubf[rc, hc], False, True, [NBR[g], Ubf])
                    cp("act", osb, osb[rc, :], PS[5][rc, :], [PS[5]])
                    for h in range(8):
                        g, j = h // 2, h % 2
                        ph = slice(64 * j, 64 * j + 64)
                        hc = slice(h * 64, h * 64 + 64)
                        mm(PS[4], PS[4][ph, g * 64:(g + 1) * 64], Kp[rc, hc], Vbf[rc, hc], True, False, [Kp, Vbf])
                        mm(PS[4], PS[4][ph, g * 64:(g + 1) * 64], Bpn[rc, hc], Ubf[rc, hc], False, True, [Bpn, Ubf])
                    tt("pool", Sdec, Sdec[:, :, :], Sf[:, :, :], PCt[:, :, c:c + 1].broadcast_to([128, 4, 64]), ALU.mult, [Sf, PCt])
                    tt("dve", Sf, Sf[:, :, :], Sdec[:, :, :], PS[4][:, 0:256].rearrange("p (g v) -> p g v", g=4), ALU.add, [Sdec, PS[4]])
                    cp("act", S_n, S_n[:, :, :], Sf[:, :, :], [Sf])
                    scur = 1 - scur
                mark(12)
                red("dve", s1t, s1t[:, :], v3(osb[:, :]), ALU.add, [osb])
                tt("pool", osq, osq[:, :], osb[:, :], osb[:, :], ALU.mult, [osb])
                red("dve", s2t, s2t[:, :], v3(osq[:, :]), ALU.add, [osq])
                ts("dve", mean, mean[:, :], s1t[:, :], 1.0 / 64, None, ALU.mult, None, [s1t])
                tt("dve", msq, msq[:, :], mean[:, :], mean[:, :], ALU.mult, [mean])
                stt("dve", var, var[:, :], s2t[:, :], 1.0 / 64, msq[:, :], ALU.mult, ALU.subtract, [s2t, msq])
                ts("dve", rstd, rstd[:, :], var[:, :], GN_EPS, None, ALU.add, None, [var])
                act(rstd, rstd[:, :], rstd[:, :], AF.Sqrt, [rstd])
                ctx.op("dve", lambda e: e.reciprocal(out=rstd[:, :], in_=rstd[:, :]), reads=[rstd], writes=[rstd])
                tt("pool", on, v3(on[:, :]), v3(osb[:, :]), mean[:, :, None].broadcast_to([128, 8, 64]), ALU.subtract, [osb, mean])
                tt("pool", on, v3(on[:, :]), v3(on[:, :]), rstd[:, :, None].broadcast_to([128, 8, 64]), ALU.mult, [on, rstd])
                tt("pool", on, on[:, :], on[:, :], PV[:, P_LG, :], ALU.mult, [on, PV])
                tt("pool", on, on[:, :], on[:, :], PV[:, P_LB, :], ALU.add, [on, PV])
                tt("pool", on, on[:, :], on[:, :], bonus[:, :], ALU.add, [on, bonus])
                if blk == 0:
                    for nm, tl_ in (("pm", pm), ("sg", sg), ("aa", aa), ("kkn", kkn), ("kmod", kmod), ("bb", bb), ("Kp", Kp), ("Bpn", Bpn),
                                    ("osb", osb), ("eL", eL), ("eLm", eLm), ("eLcL", eLcL), ("on", on), ("gsb", gsb), ("bonus", bonus)):
                        dbg(nm, tl_, tl_[:, :])
                    dbg("FMs", FMs, FMs[:, :, :]); dbg("TT0", TTb[0], TTb[0][:, :, :]); dbg("MN0", MN[0], MN[0][:, :, :])
                    dbg("NBR0", NBR[0], NBR[0][:, :, :]); dbg("Sf", Sf, Sf[:, :, :]); dbg("PCt", PCt, PCt[:, :, :]); dbg("NP0", NPX[0][0], NPX[0][0][:, :, :])
                if blk % 2 == 0:
                    tt("dve", oev, oev[:, :], on[:, :], gsb[:, :], ALU.mult, [on, gsb])
                else:
                    tt("dve", ofin, ofin[:, :], on[:, :], gsb[:, :], ALU.mult, [on, gsb])
                    ts("dve", otmp, otmp[:, :], oev[:, :], selc[:, 0:1], None, ALU.mult, None, [oev, selc])
                    oab_ = oab[(blk // 2) % 2]
                    stt("dve", oab_, oab_[:, :], ofin[:, :], selc[:, 1:2], otmp[:, :], ALU.mult, ALU.add, [ofin, selc, otmp])
                    st("sp", oa_d[(blk // 2) * 128:(blk // 2 + 1) * 128, :], oab_, oab_[:, :], dres=oa_res)
            mark(13)
            lastp = prw[(NB - 1) % 2]
            st("sp", o_shift[0:1, :], lastp, lastp[127:128, :])
            for g in range(4):
                tr(PS[0], PS[0][0:64, g * 128:(g + 1) * 128], Sf[:, g, :], ident[:, :], [Sf, ident])
            print("P1 sbuf remaining", nc.sbuf_bytes_remaining)
            cp("dve", tA, tA[0:64, :], PS[0][0:64, 0:512], [PS[0]])
            st("sp", o_wkv.rearrange("(g j) v k -> v g j k", g=4), tA, tA[0:64, :].rearrange("p (g j k) -> p g j k", g=4, j=2))
            ctx.barrier()
            ASTK.pop()


    if "p1" in cfg.phases and cfg.sample:
        with ExitStack() as s2:
            ASTK.append(s2)

            def sb2(name, shape, dt=F32):
                return TL(s2.enter_context(nc.sbuf_tensor(name, list(shape), dt)), name)
            ngr = [(0, 512), (512, 512), (1024, 512), (1536, 256)]
            P_W0, P_A0, P_KK, P_KA, P_RK, P_LG, P_LB = range(7)
            xst = sb2("xst", [16, D])
            hsT = sb2("hsT", [128, 8, 16], BF16)
            hsf = sb2("hsf", [128, 8, 16])
            prs = sb2("prs", [16, RWC])
            pms = sb2("pms", [16, RWC])
            sts = sb2("sts", [16, RWC])
            kvs_s = sb2("kvs2", [16, 2])
            smp = sb2("smp", [16, 320])
            krtmp_s = sb2("krtmp2", [16, 32])
            sqt = sb2("sqt", [16, 512])
            ld("sp", xst, xst[:, :], xs[:, :])
            ld("sp", sts, sts[:, :], st_shift[:, :])
            for kc in range(8):
                tr(PS[0], PS[0][:, kc * 16:(kc + 1) * 16], xst[:, kc * 128:(kc + 1) * 128], ident[0:16, 0:16], [xst, ident])
            tt("dve", hsf, hsf[:, :, :], PS[0][:, 0:128].rearrange("p (a b) -> p a b", a=8), MODT[:, 8:16, 1:17], ALU.mult, [PS[0], MODT])
            tt("dve", hsT, hsT[:, :, :], hsf[:, :, :], MODT[:, 0:8, 1:17], ALU.add, [hsf, MODT])
            for n, (c0, cw) in enumerate(ngr):
                pb = PS[2 + n]
                for kc in range(8):
                    mm(pb, pb[0:16, 0:cw], hsT[:, kc, :], Wrw[:, kc, c0:c0 + cw], kc == 0, kc == 7, [hsT, Wrw])
                cp("act" if n % 2 == 0 else "dve", prs, prs[:, c0:c0 + cw], pb[0:16, 0:cw], [pb])
            st("sp", o_shift_s[:, :], prs, prs[:, :])
            for kc in range(8):
                mm(PS[6], PS[6][0:16, 0:320], hsT[:, kc, :], Wkv[:, kc, :], kc == 0, kc == 7, [hsT, Wkv])
            act(sqt, sqt[:, 0:256], PS[6][0:16, 0:256], AF.Square, [PS[6]])
            red("dve", kvs_s, kvs_s[:, 0:1], sqt[:, 0:256], ALU.add, [sqt])
            ts("dve", kvs_s, kvs_s[:, 1:2], kvs_s[:, 0:1], 1.0 / 256, RMS_EPS, ALU.mult, ALU.add, [kvs_s])
            act(kvs_s, kvs_s[:, 1:2], kvs_s[:, 1:2], AF.Sqrt, [kvs_s])
            ctx.op("dve", lambda e: e.reciprocal(out=kvs_s[:, 1:2], in_=kvs_s[:, 1:2]), reads=[kvs_s], writes=[kvs_s])
            stt("dve", smp, smp[:, 0:256], PS[6][0:16, 0:256], kvs_s[:, 1:2], GKV[0:16, :], ALU.mult, ALU.mult, [PS[6], kvs_s, GKV])
            tt("dve", smp, smp[:, 256:288], PS[6][0:16, 256:288], RPs[:, 0:32], ALU.mult, [PS[6], RPs])
            tt("dve", krtmp_s, krtmp_s[:, :], PS[6][0:16, 288:320], RPs[:, 32:64], ALU.mult, [PS[6], RPs])
            tt("dve", smp, smp[:, 256:288], smp[:, 256:288], krtmp_s[:, :], ALU.add, [smp, krtmp_s])
            mset("dve", smp, smp[:, 288:320], 0.0)
            st("sp", o_smp[:, :], smp, smp[:, :])
            def v3s(ap):
                return ap.rearrange("p (h d) -> p h d", h=8)
            tt("dve", pms, pms[:, :], sts[:, :], prs[:, :], ALU.subtract, [sts, prs])
            tt("dve", pms, pms[:, :], pms[:, :], MUb[0:16, :], ALU.mult, [pms, MUb])
            tt("dve", pms, pms[:, :], pms[:, :], prs[:, :], ALU.add, [pms, prs])
            lot = sb2("lot_s", [16, 256], BF16)
            loTs = sb2("loT_s", [128, 2, 16], BF16)
            act(lot, lot[:, 0:64], pms[:, 1536:1600], AF.Tanh, [pms])
            cp("dve", lot, lot[:, 64:128], pms[:, 1600:1664], [pms])
            act(lot, lot[:, 128:256], pms[:, 1664:1792], AF.Sigmoid, [pms])
            for i in range(2):
                tr(PS[0], psb(0)[:, i * 16:(i + 1) * 16], lot[:, i * 128:(i + 1) * 128], identb[0:16, 0:16], [lot, identb])
            cp("dve", loTs, loTs[:, :, :], psb(0)[:, 0:32].rearrange("p (a b) -> p a b", a=2), [PS[0]])
            mm(PS[1], PS[1][0:16, :], loTs[0:64, 0, :], Wlo[0:64, :], True, True, [loTs, Wlo])
            mm(PS[2], PS[2][0:16, :], loTs[64:128, 0, :], Wlo[64:128, :], True, True, [loTs, Wlo])
            mm(PS[3], PS[3][0:16, :], loTs[:, 1, :], Wgu[:, :], True, True, [loTs, Wgu])
            VP = sb2("vpack", [16, 8, 512])
            tq = sb2("tq", [16, 512])
            tq2 = sb2("tq2", [16, 512])
            aas = sb2("aas", [16, 512])
            s8 = sb2("s8", [16, 8])
            r_ap, k_ap, v_ap = pms[:, 0:512], pms[:, 512:1024], pms[:, 1024:1536]
            cp("act", VP, VP[:, 0, :], r_ap, [pms])
            cp("act", VP, VP[:, 2, :], v_ap, [pms])
            tt("dve", tq, tq[:, :], PS[1][0:16, :], PV[0:16, P_W0, :], ALU.add, [PS[1], PV])
            act(tq, tq[:, :], tq[:, :], AF.Sigmoid, [tq])
            act(VP, VP[:, 5, :], tq[:, :], AF.Exp, [tq], scale=-LOGK)
            tt("dve", tq, tq[:, :], PS[2][0:16, :], PV[0:16, P_A0, :], ALU.add, [PS[2], PV])
            act(aas, aas[:, :], tq[:, :], AF.Sigmoid, [tq])
            cp("act", VP, VP[:, 7, :], PS[3][0:16, :], [PS[3]])
            tt("dve", tq, tq[:, :], k_ap, PV[0:16, P_KK, :], ALU.mult, [pms, PV])
            tt("dve", tq2, tq2[:, :], tq[:, :], tq[:, :], ALU.mult, [tq])
            red("dve", s8, s8[:, :], v3s(tq2[:, :]), ALU.add, [tq2])
            ts("dve", s8, s8[:, :], s8[:, :], 1e-12, None, ALU.add, None, [s8])
            act(s8, s8[:, :], s8[:, :], AF.Sqrt, [s8])
            ctx.op("dve", lambda e: e.reciprocal(out=s8[:, :], in_=s8[:, :]), reads=[s8], writes=[s8])
            tt("dve", VP, v3s(VP[:, 3, :]), v3s(tq[:, :]), s8[:, :, None].broadcast_to([16, 8, 64]), ALU.mult, [tq, s8])
            stt("dve", tq, tq[:, :], aas[:, :], -1.0, PV[0:16, P_KA, :], ALU.add, ALU.mult, [aas, PV])
            stt("dve", VP, VP[:, 1, :], tq[:, :], 1.0, k_ap, ALU.add, ALU.mult, [tq, pms])
            tt("dve", VP, VP[:, 4, :], VP[:, 3, :], aas[:, :], ALU.mult, [VP, aas])
            tt("dve", tq, tq[:, :], r_ap, VP[:, 1, :], ALU.mult, [pms, VP])
            tt("dve", tq, tq[:, :], tq[:, :], PV[0:16, P_RK, :], ALU.mult, [tq, PV])
            red("dve", s8, s8[:, :], v3s(tq[:, :]), ALU.add, [tq])
            tt("dve", VP, v3s(VP[:, 6, :]), v3s(v_ap), s8[:, :, None].broadcast_to([16, 8, 64]), ALU.mult, [pms, s8])
            dscr = nc.dram_tensor("dscr", [8, 16, 512], F32, kind="Internal").ap()
            dres = Res("dscr")
            st("sp", dscr.rearrange("v b n -> b v n"), VP, VP[:, :, :], dres=dres)
            vt = sb2("vt", [128, 8, 64])
            ld("sp", vt, vt[:, :, :], dscr.rearrange("v b (h d) -> (b h) v d", h=8), extra_r=[dres])
            S = sb2("S_s", [128, 64, 64])
            tmpS = sb2("tmpS", [128, 64, 64])
            us = sb2("us", [128, 64])
            osm = sb2("osm", [128, 64])
            ld("sp", S, S[:, :, :].rearrange("p v k -> p (v k)"), st_wkv[:, :])

            def bk(i):
                return vt[:, i:i + 1, :].broadcast_to([128, 64, 64])

            def bv(ap2):
                return ap2[:, :, None].broadcast_to([128, 64, 64])
            tt("dve", tmpS, tmpS[:, :, :], S[:, :, :], bk(3), ALU.mult, [S, vt])
            red("dve", us, us[:, :], tmpS[:, :, :], ALU.add, [tmpS])
            tt("pool", S, S[:, :, :], S[:, :, :], bk(5), ALU.mult, [S, vt])
            tt("dve", tmpS, tmpS[:, :, :], bv(us[:, :]), bk(4), ALU.mult, [us, vt])
            tt("pool", S, S[:, :, :], S[:, :, :], tmpS[:, :, :], ALU.subtract, [S, tmpS])
            tt("dve", tmpS, tmpS[:, :, :], bv(vt[:, 2, :]), bk(1), ALU.mult, [vt])
            tt("pool", S, S[:, :, :], S[:, :, :], tmpS[:, :, :], ALU.add, [S, tmpS])
            st("sp", o_wkv_s[:, :], S, S[:, :, :].rearrange("p v k -> p (v k)"))
            tt("dve", tmpS, tmpS[:, :, :], S[:, :, :], bk(0), ALU.mult, [S, vt])
            red("dve", osm, osm[:, :], tmpS[:, :, :], ALU.add, [tmpS])
            dscr2 = nc.dram_tensor("dscr2", [128, 64], F32, kind="Internal").ap()
            dres2 = Res("dscr2")
            st("sp", dscr2[:, :], osm, osm[:, :], dres=dres2)
            osT = sb2("osT", [16, 512])
            ld("sp", osT, osT[:, :], dscr2.rearrange("(b h) d -> b (h d)", h=8), extra_r=[dres2])
            g8 = sb2("g8", [16, 5, 8])
            red("dve", g8, g8[:, 0, :], v3s(osT[:, :]), ALU.add, [osT])
            tt("dve", tq, tq[:, :], osT[:, :], osT[:, :], ALU.mult, [osT])
            red("dve", g8, g8[:, 1, :], v3s(tq[:, :]), ALU.add, [tq])
            ts("dve", g8, g8[:, 2, :], g8[:, 0, :], 1.0 / 64, None, ALU.mult, None, [g8])
            tt("dve", g8, g8[:, 3, :], g8[:, 2, :], g8[:, 2, :], ALU.mult, [g8])
            stt("dve", g8, g8[:, 4, :], g8[:, 1, :], 1.0 / 64, g8[:, 3, :], ALU.mult, ALU.subtract, [g8])
            ts("dve", g8, g8[:, 4, :], g8[:, 4, :], GN_EPS, None, ALU.add, None, [g8])
            act(g8, g8[:, 4, :], g8[:, 4, :], AF.Sqrt, [g8])
            recip(g8, g8[:, 4, :], g8[:, 4, :], [g8])
            tt("dve", tq, v3s(tq[:, :]), v3s(osT[:, :]), g8[:, 2, :, None].broadcast_to([16, 8, 64]), ALU.subtract, [osT, g8])
            tt("dve", tq, v3s(tq[:, :]), v3s(tq[:, :]), g8[:, 4, :, None].broadcast_to([16, 8, 64]), ALU.mult, [tq, g8])
            tt("dve", tq, tq[:, :], tq[:, :], PV[0:16, P_LG, :], ALU.mult, [tq, PV])
            tt("dve", tq, tq[:, :], tq[:, :], PV[0:16, P_LB, :], ALU.add, [tq, PV])
            tt("dve", tq, tq[:, :], tq[:, :], VP[:, 6, :], ALU.add, [tq, VP])
            tt("dve", tq2, tq2[:, :], tq[:, :], VP[:, 7, :], ALU.mult, [tq, VP])
            st("sp", oas_d[:, :], tq2, tq2[:, :], dres=oas_res)
            ctx.barrier()
            ASTK.pop()


    if sM is not None:
        ctx.barrier()
        sM.close()
    ob_d = nc.dram_tensor("ob_d", [cfg.NBO * 128, 512], F32, kind="Internal").ap()
    ob_res = Res("ob_d")

    if "p2" in cfg.phases:
        xo = din("xo", [cfg.NBO * 128, D])
        ropeo = din("ropeo", [cfg.NBO * 128, 64])
        w_q = din("w_q", [D, 384])
        gq = din("gq", [1, 384])
        w_uqx = din("w_uqx", [384, 1024])
        w_ukp = din("w_ukp", [256, 8 * 96])
        w_uv = din("w_uv", [256, 512])
        c_cmask = din("cmask", [128, 256])
        with ExitStack() as s3:
            ASTK.append(s3)

            def sb3(name, shape, dt=F32):
                return TL(s3.enter_context(nc.sbuf_tensor(name, list(shape), dt)), name)
            KH = sb3("KH", [96, 8, T], BF16)
            VA = sb3("VA", [128, NB, 8, 65], BF16)
            QT = sb3("QT", [96, 8, 128], BF16)
            Wq = sb3("Wq", [128, 8, 384], BF16)
            Wuqx = sb3("Wuqx", [128, 3, 1024], BF16)
            Wukp = sb3("Wukp", [128, 2, 768], BF16)
            Wuv = sb3("Wuv", [128, 2, 512], BF16)
            GQ = sb3("GQ", [128, 384])
            CM = sb3("CM", [128, 2, 128], BF16)
            ld("pool", Wq, Wq[:, :, :], w_q.rearrange("(kc p) n -> p kc n", p=128))
            ld("pool", Wuqx, Wuqx[:, :, :], w_uqx.rearrange("(kc p) n -> p kc n", p=128))
            ld("pool", Wukp, Wukp[:, :, :], w_ukp.rearrange("(kc p) n -> p kc n", p=128))
            ld("pool", Wuv, Wuv[:, :, :], w_uv.rearrange("(kc p) n -> p kc n", p=128))
            ld("sp", GQ, GQ[:, :], gq.partition_broadcast(128))
            ld("pool", CM, CM[:, :, :].rearrange("p a b -> p (a b)"), c_cmask[:, :])
            mset("dve", VA, VA[:, :, :, :], 1.0)
            ckf = [sb3("ckf%d" % i, [128, 256]) for i in range(2)]
            krf = [sb3("krf%d" % i, [128, 32]) for i in range(2)]
            ckb = sb3("ckb", [128, 256], BF16)
            krp = sb3("krp", [128, 96], BF16)
            ckT = sb3("ckT", [128, 2, 128], BF16)
            mset("dve", krp, krp[:, :], 0.0)
            ores_ckv = Res("o_ckv_res")
            for kb in range(NB):
                c_ = kb % 2
                ld("sp", ckf[c_], ckf[c_][:, :], o_ckv[kb * 128:(kb + 1) * 128, :])
                ld("sp", krf[c_], krf[c_][:, :], o_kr[kb * 128:(kb + 1) * 128, :])
                cp("act", ckb, ckb[:, :], ckf[c_][:, :], [ckf[c_]])
                cp("dve", krp, krp[:, 64:96], krf[c_][:, :], [krf[c_]])
                for cc in range(2):
                    tr(PS[0], psb(0)[:, cc * 128:(cc + 1) * 128], ckb[:, cc * 128:(cc + 1) * 128], identb[:, :], [ckb, identb])
                cp("dve", ckT, ckT[:, :, :], psb(0)[:, 0:256].rearrange("p (a b) -> p a b", a=2), [PS[0]])
                for h in range(8):
                    pb = PS[1 + h // 4]
                    oap = pb[0:96, (h % 4) * 128:(h % 4 + 1) * 128]
                    mm(pb, oap, Wukp[:, 0, h * 96:(h + 1) * 96], ckT[:, 0, :], True, False, [Wukp, ckT])
                    mm(pb, oap, Wukp[:, 1, h * 96:(h + 1) * 96], ckT[:, 1, :], False, False, [Wukp, ckT])
                    mm(pb, oap, krp[:, :], identb[:, :], False, True, [krp, identb])
                for half in range(2):
                    pb = PS[1 + half]
                    cp("act" if half == 0 else "dve", KH, KH[:, 4 * half:4 * half + 4, kb * 128:(kb + 1) * 128],
                       pb[0:96, :].rearrange("p (h t) -> p h t", h=4), [pb])
                for cc in range(2):
                    mm(PS[3], PS[3][:, :], ckT[:, cc, :], Wuv[:, cc, :], cc == 0, cc == 1, [ckT, Wuv])
                cp("act", VA, VA[:, kb, :, 0:64], PS[3][:, :].rearrange("p (h d) -> p h d", h=8), [PS[3]])
            xo_t = [sb3("xo_t%d" % i, [128, D]) for i in range(2)]
            rpo = [sb3("rpo%d" % i, [128, 64]) for i in range(2)]
            hoT = sb3("hoT", [128, 8, 128], BF16)
            qsq = sb3("qsq", [128, 384])
            qs2 = sb3("qs2", [128, 2])
            cq = sb3("cq", [128, 384], BF16)
            cqT = sb3("cqT", [128, 3, 128], BF16)
            qf = sb3("qf", [128, 1024])
            qr1 = sb3("qr1", [128, 8, 32])
            qtm = sb3("qtm", [128, 8, 96], BF16)
            PT = [sb3("PT%d" % i, [128, 4, 128], BF16) for i in range(2)]
            rsum = sb3("rsum", [128, 8])
            accs = sb3("accs", [128, 8, 65])
            obo_t = [sb3("obo%d" % i, [128, 512]) for i in range(2)]
            pti = 0
            for jj in range(cfg.NBO):
                c_ = jj % 2
                ld("sp", xo_t[c_], xo_t[c_][:, :], xo[jj * 128:(jj + 1) * 128, :])
                ld("sp", rpo[c_], rpo[c_][:, :], ropeo[jj * 128:(jj + 1) * 128, :])
                for kc in range(8):
                    pb = PS[kc // 4]
                    tr(pb, pb[:, (kc % 4) * 128:(kc % 4 + 1) * 128], xo_t[c_][:, kc * 128:(kc + 1) * 128], ident[:, :], [xo_t[c_], ident])
                for kc in range(8):
                    pb = PS[kc // 4]
                    act(hoT, hoT[:, kc, :], pb[:, (kc % 4) * 128:(kc % 4 + 1) * 128], AF.Identity, [pb, MODT],
                        bias=MODT[:, kc, 0:1], scale=MODT[:, 8 + kc, 0:1])
                for kc in range(8):
                    mm(PS[2], PS[2][:, 0:384], hoT[:, kc, :], Wq[:, kc, :], kc == 0, kc == 7, [hoT, Wq])
                act(qsq, qsq[:, :], PS[2][:, 0:384], AF.Square, [PS[2]])
                red("dve", qs2, qs2[:, 0:1], qsq[:, :], ALU.add, [qsq])
                ts("dve", qs2, qs2[:, 1:2], qs2[:, 0:1], 1.0 / 384, RMS_EPS, ALU.mult, ALU.add, [qs2])
                act(qs2, qs2[:, 1:2], qs2[:, 1:2], AF.Sqrt, [qs2])
                recip(qs2, qs2[:, 1:2], qs2[:, 1:2], [qs2])
                stt("dve", cq, cq[:, :], PS[2][:, 0:384], qs2[:, 1:2], GQ[:, :], ALU.mult, ALU.mult, [PS[2], qs2, GQ])
                for i in range(3):
                    tr(PS[3], psb(3)[:, i * 128:(i + 1) * 128], cq[:, i * 128:(i + 1) * 128], identb[:, :], [cq, identb])
                cp("dve", cqT, cqT[:, :, :], psb(3)[:, 0:384].rearrange("p (a b) -> p a b", a=3), [PS[3]])
                for nb_ in range(2):
                    pb = PS[4 + nb_]
                    for i in range(3):
                        mm(pb, pb[:, :], cqT[:, i, :], Wuqx[:, i, nb_ * 512:(nb_ + 1) * 512], i == 0, i == 2, [cqT, Wuqx])
                    cp("act" if nb_ == 0 else "dve", qf, qf[:, nb_ * 512:(nb_ + 1) * 512], pb[:, :], [pb])
                q3 = qf[:, 0:768].rearrange("p (h d) -> p h d", h=8)
                qsw = qf[:, 768:1024].rearrange("p (h d) -> p h d", h=8)
                cosb = rpo[c_][:, None, 0:32].broadcast_to([128, 8, 32])
                sinb = rpo[c_][:, None, 32:64].broadcast_to([128, 8, 32])
                cp("act", qtm, qtm[:, :, 0:64], q3[:, :, 0:64], [qf])
                tt("dve", qr1, qr1[:, :, :], q3[:, :, 64:96], cosb, ALU.mult, [qf, rpo[c_]])
                tt("pool", qf, qsw, qsw, sinb, ALU.mult, [qf, rpo[c_]])
                tt("dve", qtm, qtm[:, :, 64:96], qr1[:, :, :], qsw, ALU.add, [qr1, qf])
                for h in range(8):
                    pb = PS[h // 4]
                    tr(pb, psb(h // 4)[0:96, (h % 4) * 128:(h % 4 + 1) * 128], qtm[:, h, :], identb[:, :], [qtm, identb])
                for half in range(2):
                    cp("act" if half == 0 else "dve", QT, QT[:, 4 * half:4 * half + 4, :],
                       psb(half)[0:96, 0:512].rearrange("p (h t) -> p h t", h=4), [PS[half]])
                nkb = 2 * jj + 2
                groups = [(g0, min(4, nkb - g0)) for g0 in range(0, nkb, 4)]
                for h in range(8):
                    pacc = PS[6 + h // 4]
                    acc_ap = pacc[:, (h % 4) * 65:(h % 4) * 65 + 65]
                    for gi, (g0, gn) in enumerate(groups):
                        pbs = PS[2 + pti % 2]
                        pt = PT[pti % 2]
                        pti += 1
                        for i in range(gn):
                            kb = g0 + i
                            mm(pbs, pbs[:, i * 128:(i + 1) * 128], KH[:, h, kb * 128:(kb + 1) * 128], QT[:, h, :], True, True, [KH, QT])
                        act(pt, pt[:, 0:gn, :], pbs[:, 0:gn * 128].rearrange("p (a b) -> p a b", a=gn), AF.Exp, [pbs], scale=ATTN_SCALE)
                        for i in range(gn):
                            kb = g0 + i
                            if kb >= nkb - 2:
                                tt("pool", pt, pt[:, i, :], pt[:, i, :], CM[:, kb - (nkb - 2), :], ALU.mult, [pt, CM])
                        for i in range(gn):
                            kb = g0 + i
                            mm(pacc, acc_ap, pt[:, i, :], VA[:, kb, h, :], kb == 0, kb == nkb - 1, [pt, VA])
                for half in range(2):
                    cp("act" if half == 0 else "dve", accs, accs[:, 4 * half:4 * half + 4, :],
                       PS[6 + half][:, 0:260].rearrange("p (h d) -> p h d", h=4), [PS[6 + half]])
                recip(rsum, rsum[:, :], accs[:, :, 64], [accs])
                obo = obo_t[jj % 2]
                tt("dve", obo, obo[:, :].rearrange("p (h d) -> p h d", h=8), accs[:, :, 0:64],
                   rsum[:, :, None].broadcast_to([128, 8, 64]), ALU.mult, [accs, rsum])
                st("sp", ob_d[jj * 128:(jj + 1) * 128, :], obo, obo[:, :], dres=ob_res)
            ctx.barrier()
            ASTK.pop()


    if "p3" in cfg.phases:
        NPG = cfg.NPG
        cache_c = din("cache_c", [cfg.NPHYS * 128, 256])
        cache_k = din("cache_k", [cfg.NPHYS * 128, 32])
        ptab = din("ptab", [1, 16 * NPG], I32)
        with ExitStack() as s5:
            ASTK.append(s5)

            def sb5(name, shape, dt=F32):
                return TL(s5.enter_context(nc.sbuf_tensor(name, list(shape), dt)), name)
            Wq5 = sb5("Wq5", [128, 8, 384], BF16)
            Wuqx5 = sb5("Wuqx5", [128, 3, 1024], BF16)
            Wukp5 = sb5("Wukp5", [128, 2, 768], BF16)
            Wuv5 = sb5("Wuv5", [128, 2, 512], BF16)
            GQ5 = sb5("GQ5", [16, 384])
            RP5 = sb5("RP5", [16, 64])
            ld("pool", Wq5, Wq5[:, :, :], w_q.rearrange("(kc p) n -> p kc n", p=128))
            ld("pool", Wuqx5, Wuqx5[:, :, :], w_uqx.rearrange("(kc p) n -> p kc n", p=128))
            ld("pool", Wukp5, Wukp5[:, :, :], w_ukp.rearrange("(kc p) n -> p kc n", p=128))
            ld("pool", Wuv5, Wuv5[:, :, :], w_uv.rearrange("(kc p) n -> p kc n", p=128))
            ld("sp", GQ5, GQ5[:, :], gq.partition_broadcast(16))
            ld("sp", RP5, RP5[:, :], ropes.partition_broadcast(16))
            PTB = sb5("PTB", [128, 16 * NPG], I32)
            IDX = sb5("IDX", [128, 16 * NPG], I32)
            IOT = sb5("IOT", [128, 1], I32)
            ld("sp", PTB, PTB[:, :], ptab.partition_broadcast(128))
            ctx.op("pool", lambda e: e.iota(IOT[:, :], pattern=[[0, 1]], base=0, channel_multiplier=1), reads=[], writes=[IOT])
            ctx.op("dve", lambda e: e.tensor_scalar(out=IDX[:, :], in0=PTB[:, :], scalar1=128, scalar2=IOT[:, 0:1], op0=ALU.mult, op1=ALU.add), reads=[PTB, IOT], writes=[IDX])
            xs5 = sb5("xs5", [16, D])
            hs5f = sb5("hs5f", [128, 8, 16])
            hs5 = sb5("hs5", [128, 8, 16], BF16)
            q5s = sb5("q5s", [16, 384])
            q5n = sb5("q5n", [16, 2])
            cq5 = sb5("cq5", [16, 384], BF16)
            cq5T = sb5("cq5T", [128, 3, 16], BF16)
            qf5 = sb5("qf5", [16, 1024])
            qr5 = sb5("qr5", [16, 8, 32])
            qtm5 = sb5("qtm5", [16, 8, 96], BF16)
            QT5 = sb5("QT5", [96, 8, 16], BF16)
            ld("sp", xs5, xs5[:, :], xs[:, :])
            for kc in range(8):
                tr(PS[0], PS[0][:, kc * 16:(kc + 1) * 16], xs5[:, kc * 128:(kc + 1) * 128], ident[0:16, 0:16], [xs5, ident])
            tt("dve", hs5f, hs5f[:, :, :], PS[0][:, 0:128].rearrange("p (a b) -> p a b", a=8), MODT[:, 8:16, 1:17], ALU.mult, [PS[0], MODT])
            tt("dve", hs5, hs5[:, :, :], hs5f[:, :, :], MODT[:, 0:8, 1:17], ALU.add, [hs5f, MODT])
            for kc in range(8):
                mm(PS[2], PS[2][0:16, 0:384], hs5[:, kc, :], Wq5[:, kc, :], kc == 0, kc == 7, [hs5, Wq5])
            act(q5s, q5s[:, :], PS[2][0:16, 0:384], AF.Square, [PS[2]])
            red("dve", q5n, q5n[:, 0:1], q5s[:, :], ALU.add, [q5s])
            ts("dve", q5n, q5n[:, 1:2], q5n[:, 0:1], 1.0 / 384, RMS_EPS, ALU.mult, ALU.add, [q5n])
            act(q5n, q5n[:, 1:2], q5n[:, 1:2], AF.Sqrt, [q5n])
            recip(q5n, q5n[:, 1:2], q5n[:, 1:2], [q5n])
            stt("dve", cq5, cq5[:, :], PS[2][0:16, 0:384], q5n[:, 1:2], GQ5[:, :], ALU.mult, ALU.mult, [PS[2], q5n, GQ5])
            for i in range(3):
                tr(PS[3], psb(3)[:, i * 16:(i + 1) * 16], cq5[:, i * 128:(i + 1) * 128], identb[0:16, 0:16], [cq5, identb])
            cp("dve", cq5T, cq5T[:, :, :], psb(3)[:, 0:48].rearrange("p (a b) -> p a b", a=3), [PS[3]])
            for nb_ in range(2):
                pb = PS[4 + nb_]
                for i in range(3):
                    mm(pb, pb[0:16, :], cq5T[:, i, :], Wuqx5[:, i, nb_ * 512:(nb_ + 1) * 512], i == 0, i == 2, [cq5T, Wuqx5])
                cp("act" if nb_ == 0 else "dve", qf5, qf5[:, nb_ * 512:(nb_ + 1) * 512], pb[0:16, :], [pb])
            q3_5 = qf5[:, 0:768].rearrange("p (h d) -> p h d", h=8)
            qsw5 = qf5[:, 768:1024].rearrange("p (h d) -> p h d", h=8)
            cp("act", qtm5, qtm5[:, :, 0:64], q3_5[:, :, 0:64], [qf5])
            tt("dve", qr5, qr5[:, :, :], q3_5[:, :, 64:96], RP5[:, None, 0:32].broadcast_to([16, 8, 32]), ALU.mult, [qf5, RP5])
            tt("dve", qf5, qsw5, qsw5, RP5[:, None, 32:64].broadcast_to([16, 8, 32]), ALU.mult, [qf5, RP5])
            tt("dve", qtm5, qtm5[:, :, 64:96], qr5[:, :, :], qsw5, ALU.add, [qr5, qf5])
            for h in range(8):
                tr(PS[h // 4], psb(h // 4)[0:96, (h % 4) * 16:(h % 4 + 1) * 16], qtm5[:, h, :], identb[0:16, 0:16], [qtm5, identb])
            for half in range(2):
                cp("act" if half == 0 else "dve", QT5, QT5[:, 4 * half:4 * half + 4, :],
                   psb(half)[0:96, 0:64].rearrange("p (h t) -> p h t", h=4), [PS[half]])
            ckb5 = [sb5("ckb5_%d" % i, [128, 256], BF16) for i in range(2)]
            krp5 = [sb5("krp5_%d" % i, [128, 96], BF16) for i in range(2)]
            ckT5 = sb5("ckT5", [128, 2, 128], BF16)
            KH5 = sb5("KH5", [96, 8, 128], BF16)
            VA5 = sb5("VA5", [128, 8, 65], BF16)
            PT5 = sb5("PT5", [128, 8], BF16)
            MK1 = sb5("MK1", [128, 8], BF16)
            zcol = sb5("zcol", [128, 1], BF16)
            arow = sb5("arow", [1, 8, 65])
            stg5 = sb5("stg5", [1, 320])
            rrow = sb5("rrow", [1, 8])
            obrow = sb5("obrow", [1, 512])
            for i in range(2):
                mset("dve", krp5[i], krp5[i][:, :], 0.0)
                mset("dve", ckb5[i], ckb5[i][:, :], 0.0)
            mset("dve", VA5, VA5[:, :, :], 1.0)
            mset("dve", MK1, MK1[:, :], 0.0)
            mset("dve", MK1, MK1[0:1, :], 1.0)
            mset("dve", zcol, zcol[:, :], 0.0)
            cflat = cache_c
            kflat = cache_k
            pgi = 0
            for b in range(16):
                for half in range(2):
                    mm(PS[6 + half], PS[6 + half][0:1, 0:260], zcol[:, 0:1], VA5[:, 4 * half:4 * half + 4, :].rearrange("p h d -> p (h d)"), True, False, [zcol, VA5])
                for j in range(NPG + 1):
                    c_ = pgi % 2
                    pgi += 1
                    ck_, kr_ = ckb5[c_], krp5[c_]
                    if j < NPG:
                        col = b * NPG + j
                        ctx.op("pool", (lambda e, o_=ck_[:, :], ix=IDX[:, col:col + 1]: e.indirect_dma_start(out=o_, out_offset=None, in_=cflat,
                               in_offset=bass.IndirectOffsetOnAxis(ap=ix, axis=0))), reads=[IDX], writes=[ck_], dma=True)
                        ctx.op("pool", (lambda e, o_=kr_[:, 64:96], ix=IDX[:, col:col + 1]: e.indirect_dma_start(out=o_, out_offset=None, in_=kflat,
                               in_offset=bass.IndirectOffsetOnAxis(ap=ix, axis=0))), reads=[IDX], writes=[kr_], dma=True)
                    else:
                        ld("sp", stg5, stg5[0:1, :], o_smp[b:b + 1, :])
                        cp("dve", ck_, ck_[0:1, :], stg5[0:1, 0:256], [stg5])
                        cp("dve", kr_, kr_[0:1, 64:96], stg5[0:1, 256:288], [stg5])
                    for cc in range(2):
                        tr(PS[0], psb(0)[:, cc * 128:(cc + 1) * 128], ck_[:, cc * 128:(cc + 1) * 128], identb[:, :], [ck_, identb])
                    cp("dve", ckT5, ckT5[:, :, :], psb(0)[:, 0:256].rearrange("p (a b) -> p a b", a=2), [PS[0]])
                    for h in range(8):
                        pb = PS[1 + h // 4]
                        oap = pb[0:96, (h % 4) * 128:(h % 4 + 1) * 128]
                        mm(pb, oap, Wukp5[:, 0, h * 96:(h + 1) * 96], ckT5[:, 0, :], True, False, [Wukp5, ckT5])
                        mm(pb, oap, Wukp5[:, 1, h * 96:(h + 1) * 96], ckT5[:, 1, :], False, False, [Wukp5, ckT5])
                        mm(pb, oap, kr_[:, :], identb[:, :], False, True, [kr_, identb])
                    for half in range(2):
                        pb = PS[1 + half]
                        cp("act" if half == 0 else "dve", KH5, KH5[:, 4 * half:4 * half + 4, :], pb[0:96, :].rearrange("p (h t) -> p h t", h=4), [pb])
                    for cc in range(2):
                        mm(PS[3], PS[3][:, :], ckT5[:, cc, :], Wuv5[:, cc, :], cc == 0, cc == 1, [ckT5, Wuv5])
                    cp("act", VA5, VA5[:, :, 0:64], PS[3][:, :].rearrange("p (h d) -> p h d", h=8), [PS[3]])
                    for h in range(8):
                        mm(PS[4], PS[4][:, h:h + 1], KH5[:, h, :], QT5[:, h, b:b + 1], True, True, [KH5, QT5])
                    act(PT5, PT5[:, :], PS[4][:, 0:8], AF.Exp, [PS[4]], scale=ATTN_SCALE)
                    if j == NPG:
                        tt("dve", PT5, PT5[:, :], PT5[:, :], MK1[:, :], ALU.mult, [PT5, MK1])
                    for h in range(8):
                        pacc = PS[6 + h // 4]
                        mm(pacc, pacc[0:1, (h % 4) * 65:(h % 4) * 65 + 65], PT5[:, h:h + 1], VA5[:, h, :], False, (j == NPG and h % 4 == 3), [PT5, VA5])
                for half in range(2):
                    cp("act" if half == 0 else "dve", arow, arow[:, 4 * half:4 * half + 4, :],
                       PS[6 + half][0:1, 0:260].rearrange("p (h d) -> p h d", h=4), [PS[6 + half]])
                recip(rrow, rrow[:, :], arow[:, :, 64], [arow])
                tt("dve", obrow, obrow[:, :].rearrange("p (h d) -> p h d", h=8), arow[:, :, 0:64], rrow[:, :, None].broadcast_to([1, 8, 64]), ALU.mult, [arow, rrow])
                st("sp", obs_d[b:b + 1, :], obrow, obrow[:, :], dres=obs_res)
            if cfg.debug_ob:
                obt = sb5("obt", [16, 512])
                ld("sp", obt, obt[:, :], obs_d[:, :], extra_r=[obs_res])
                o_dbgs = dout("o_dbgs", [16, 512])
                st("sp", o_dbgs[:, :], obt, obt[:, :])
            ctx.barrier()
            ASTK.pop()

    if "p4" in cfg.phases:
        NBO = cfg.NBO
        SMP = "p3" in cfg.phases
        NB4 = NBO + (1 if SMP else 0)
        NTOK = NB4 * 128
        w_g = din("w_g", [D, 2048])
        w_ba = din("w_ba", [512, D])
        w_bb = din("w_bb", [512, D])
        w_o = din("w_o", [D, D])
        lnp = din("lnp", [1, 4 * D])
        w_r = din("w_r", [D, 64])
        b_r = din("b_r", [1, 64])
        w_eg = din("w_eg", [cfg.NE, D, 256])
        w_eu = din("w_eu", [cfg.NE, D, 256])
        w_ed = din("w_ed", [cfg.NE, 256, D])
        o_y = dout("o_y", [NBO * 128, D])
        o_ys = dout("o_ys", [128, D])
        x1s = nc.dram_tensor("x1s", [NTOK, D], F32, kind="Internal").ap()
        x1res = Res("x1s")
        with ExitStack() as s4:
            ASTK.append(s4)

            def sb4(name, shape, dt=F32):
                return TL(s4.enter_context(nc.sbuf_tensor(name, list(shape), dt)), name)
            H2T = sb4("H2T", [128, 8, NTOK], BF16)
            RW = sb4("RW", [128, NB4, 65])
            LNP = sb4("LNP", [128, 4, D])
            BC = sb4("BC", [128, 4, D])
            BR = sb4("BR", [128, 64])
            ld("sp", LNP, LNP[:, :, :].rearrange("p a b -> p (a b)"), lnp.partition_broadcast(128))
            ld("sp", BR, BR[:, :], b_r.partition_broadcast(128))
            for i, ch in enumerate((16, 32, 24, 40)):
                ld("sp", BC, BC[:, i, :], modr_d[0:1, ch * 128:ch * 128 + 1024].partition_broadcast(128), extra_r=[modr_res])
            ts("dve", BC, BC[:, 1, :], BC[:, 1, :], 1.0, None, ALU.add, None, [BC])
            mset("dve", RW, RW[:, :, 64:65], 1.0)
            with ExitStack() as s4a:
                ASTK.append(s4a)

                def sb4a(name, shape, dt=F32):
                    return TL(s4a.enter_context(nc.sbuf_tensor(name, list(shape), dt)), name)
                Wg = sb4a("Wg", [128, 8, 2048], BF16)
                Wba = sb4a("Wba", [128, 4, D], BF16)
                Wbb = sb4a("Wbb", [128, 4, D], BF16)
                Wo = sb4a("Wo", [128, 8, D], BF16)
                Wr = sb4a("Wr", [128, 8, 64])
                ld("pool", Wg, Wg[:, :, :], w_g.rearrange("(kc p) n -> p kc n", p=128))
                ld("pool", Wba, Wba[:, :, :], w_ba.rearrange("(kc p) n -> p kc n", p=128))
                ld("pool", Wbb, Wbb[:, :, :], w_bb.rearrange("(kc p) n -> p kc n", p=128))
                ld("pool", Wo, Wo[:, :, :], w_o.rearrange("(kc p) n -> p kc n", p=128))
                ld("sp", Wr, Wr[:, :, :], w_r.rearrange("(kc p) n -> p kc n", p=128))
                x4s = sb4a("x4_0", [128, D])
                x4 = [x4s, x4s]
                h4T = sb4a("h4T", [128, 8, 128], BF16)
                oaj = sb4a("oaj", [128, 512], BF16)
                oaj32 = sb4a("oaj32", [128, 512])
                obj32 = sb4a("obj32", [128, 512])
                obj = sb4a("obj", [128, 512], BF16)
                hsf4 = sb4a("hsf4", [128, 8, 16])
                sga = sb4a("sga", [128, 2048])
                oT = sb4a("oT", [128, 8, 128], BF16)
                mg = sb4a("mg", [128, D])
                mgb = sb4a("mgb", [128, D], BF16)
                mT = sb4a("mT", [128, 8, 128], BF16)
                pre = sb4a("pre", [128, D])
                sqv = sb4a("sqv", [128, D])
                st4 = sb4a("st4", [128, 8])
                h2 = sb4a("h2", [128, D])
                h2Tf = sb4a("h2Tf", [128, 8, 128])
                sc = sb4a("sc", [128, 64])
                sbx = sb4a("sbx", [128, 64])
                m8 = sb4a("m8", [128, 8, 8])
                gsc = sb4a("gsc", [128, 8])
                m8g = sb4a("m8g", [128, 8])
                gmk = sb4a("gmk", [128, 8])
                gof = sb4a("gof", [128, 8])
                sbm = sb4a("sbm", [128, 64])
                m8e = sb4a("m8e", [128, 8])
                sel = sb4a("sel", [128, 64])
                wsum = sb4a("wsum", [128, 2])

                def layer_norm(src_tl, dst_tl, dst_ap, gi):
                    red("dve", st4, st4[:, 0:1], src_tl[:, :], ALU.add, [src_tl])
                    act(sqv, sqv[:, :], src_tl[:, :], AF.Square, [src_tl])
                    red("dve", st4, st4[:, 1:2], sqv[:, :], ALU.add, [sqv])
                    ts("dve", st4, st4[:, 2:3], st4[:, 0:1], 1.0 / D, None, ALU.mult, None, [st4])
                    tt("dve", st4, st4[:, 3:4], st4[:, 2:3], st4[:, 2:3], ALU.mult, [st4])
                    stt("dve", st4, st4[:, 4:5], st4[:, 1:2], 1.0 / D, st4[:, 3:4], ALU.mult, ALU.subtract, [st4])
                    ts("dve", st4, st4[:, 4:5], st4[:, 4:5], LN_EPS, None, ALU.add, None, [st4])
                    act(st4, st4[:, 4:5], st4[:, 4:5], AF.Sqrt, [st4])
                    recip(st4, st4[:, 4:5], st4[:, 4:5], [st4])
                    ts("dve", sqv, sqv[:, :], src_tl[:, :], st4[:, 2:3], None, ALU.subtract, None, [src_tl, st4])
                    ts("dve", sqv, sqv[:, :], sqv[:, :], st4[:, 4:5], None, ALU.mult, None, [sqv, st4])
                    tt("pool", sqv, sqv[:, :], sqv[:, :], LNP[:, gi, :], ALU.mult, [sqv, LNP])
                    tt("pool", dst_tl, dst_ap, sqv[:, :], LNP[:, gi + 1, :], ALU.add, [sqv, LNP])

                for jj in range(NB4):
                    c_ = jj % 2
                    x_ = x4[c_]
                    is_s = jj == NBO
                    BCx = BC
                    if is_s:
                        mset("dve", BC, BC[:, 0:3, :], 0.0)
                        for i, ch in enumerate((16, 32, 24)):
                            ld("sp", BC, BC[0:16, i, :], modrs_d[:, ch * 128:ch * 128 + 1024], extra_r=[modrs_res])
                        ts("dve", BC, BC[0:16, 1, :], BC[0:16, 1, :], 1.0, None, ALU.add, None, [BC])
                        mset("dve", x_, x_[:, :], 0.0)
                        ld("sp", x_, x_[0:16, :], xs[:, :])
                        mset("dve", h4T, h4T[:, :, :], 0.0)
                        for kc in range(8):
                            tr(PS[0], PS[0][:, kc * 16:(kc + 1) * 16], x_[0:16, kc * 128:(kc + 1) * 128], ident[0:16, 0:16], [x_, ident])
                        tt("dve", hsf4, hsf4[:, :, :], PS[0][:, 0:128].rearrange("p (a b) -> p a b", a=8), MODT[:, 8:16, 1:17], ALU.mult, [PS[0], MODT])
                        tt("dve", h4T, h4T[:, :, 0:16], hsf4[:, :, :], MODT[:, 0:8, 1:17], ALU.add, [hsf4, MODT])
                    else:
                        ld("sp", x_, x_[:, :], xo[jj * 128:(jj + 1) * 128, :])
                        for kc in range(8):
                            pb = PS[kc // 4]
                            tr(pb, pb[:, (kc % 4) * 128:(kc % 4 + 1) * 128], x_[:, kc * 128:(kc + 1) * 128], ident[:, :], [x_, ident])
                        for kc in range(8):
                            pb = PS[kc // 4]
                            act(h4T, h4T[:, kc, :], pb[:, (kc % 4) * 128:(kc % 4 + 1) * 128], AF.Identity, [pb, MODT],
                                bias=MODT[:, kc, 0:1], scale=MODT[:, 8 + kc, 0:1])
                    for n in range(4):
                        pb = PS[2 + n]
                        for kc in range(8):
                            mm(pb, pb[:, :], h4T[:, kc, :], Wg[:, kc, n * 512:(n + 1) * 512], kc == 0, kc == 7, [h4T, Wg])
                        act(sga, sga[:, n * 512:(n + 1) * 512], pb[:, :], AF.Sigmoid, [pb])
                    mark(20)
                    if is_s:
                        mset("dve", oaj32, oaj32[:, :], 0.0)
                        ld("sp", oaj32, oaj32[0:16, :], oas_d[:, :], extra_r=[oas_res])
                        mset("dve", obj32, obj32[:, :], 0.0)
                        ld("sp", obj32, obj32[0:16, :], obs_d[:, :], extra_r=[obs_res])
                        cp("act", obj, obj[:, :], obj32[:, :], [obj32])
                    else:
                        ld("sp", oaj32, oaj32[:, :], oa_d[jj * 128:(jj + 1) * 128, :], extra_r=[oa_res])
                        ld("sp", obj32, obj32[:, :], ob_d[jj * 128:(jj + 1) * 128, :], extra_r=[ob_res])
                        cp("act", obj, obj[:, :], obj32[:, :], [obj32])
                    cp("act", oaj, oaj[:, :], oaj32[:, :], [oaj32])
                    for bi, (OX, WX) in enumerate(((oaj, Wba), (obj, Wbb))):
                        for i in range(4):
                            src_ = OX[:, i * 128:(i + 1) * 128]
                            tr(PS[0], psb(0)[:, i * 128:(i + 1) * 128], src_, identb[:, :], [OX, identb])
                        cp("dve", oT, oT[:, 4 * bi:4 * bi + 4, :], psb(0)[:, 0:512].rearrange("p (a b) -> p a b", a=4), [PS[0]])
                        for n in range(2):
                            pb = PS[2 + 2 * bi + n]
                            for i in range(4):
                                mm(pb, pb[:, :], oT[:, 4 * bi + i, :], WX[:, i, n * 512:(n + 1) * 512], i == 0, i == 3, [oT, WX])
                    mark(30)
                    for n in range(2):
                        tt("dve", mg, mg[:, n * 512:(n + 1) * 512], PS[2 + n][:, :], sga[:, n * 512:(n + 1) * 512], ALU.mult, [PS[2 + n], sga])
                        tt("dve", pre, pre[:, n * 512:(n + 1) * 512], PS[4 + n][:, :], sga[:, 1024 + n * 512:1024 + (n + 1) * 512], ALU.mult, [PS[4 + n], sga])
                    tt("pool", mgb, mgb[:, :], mg[:, :], pre[:, :], ALU.add, [mg, pre])
                    for kc in range(8):
                        tr(PS[kc // 4], psb(kc // 4)[:, (kc % 4) * 128:(kc % 4 + 1) * 128], mgb[:, kc * 128:(kc + 1) * 128], identb[:, :], [mgb, identb])
                    for half in range(2):
                        cp("act" if half == 0 else "dve", mT, mT[:, 4 * half:4 * half + 4, :], psb(half)[:, 0:512].rearrange("p (a b) -> p a b", a=4), [PS[half]])
                    for n in range(2):
                        pb = PS[2 + n]
                        for kc in range(8):
                            mm(pb, pb[:, :], mT[:, kc, :], Wo[:, kc, n * 512:(n + 1) * 512], kc == 0, kc == 7, [mT, Wo])
                        tt("dve", pre, pre[:, n * 512:(n + 1) * 512], pb[:, :], BCx[:, 0, n * 512:(n + 1) * 512], ALU.mult, [pb, BCx])
                    mark(31)
                    stt("dve", pre, pre[:, :], x_[:, :], ALPHA, pre[:, :], ALU.mult, ALU.add, [x_, pre])
                    layer_norm(pre, mg, mg[:, :], 0)
                    st("sp", x1s[jj * 128:(jj + 1) * 128, :], mg, mg[:, :], dres=x1res)
                    mark(32)
                    tt("dve", h2, h2[:, :], mg[:, :], BCx[:, 1, :], ALU.mult, [mg, BCx])
                    tt("pool", h2, h2[:, :], h2[:, :], BCx[:, 2, :], ALU.add, [h2, BCx])
                    for kc in range(8):
                        pb = PS[kc // 4]
                        tr(pb, pb[:, (kc % 4) * 128:(kc % 4 + 1) * 128], h2[:, kc * 128:(kc + 1) * 128], ident[:, :], [h2, ident])
                    mark(33)
                    for half in range(2):
                        cp("act", h2Tf, h2Tf[:, 4 * half:4 * half + 4, :], PS[half][:, :].rearrange("p (a b) -> p a b", a=4), [PS[half]])
                        cp("dve", H2T, H2T[:, 4 * half:4 * half + 4, jj * 128:(jj + 1) * 128], h2Tf[:, 4 * half:4 * half + 4, :], [h2Tf])
                    mark(21)
                    for kc in range(8):
                        mm(PS[2], PS[2][:, 0:64], h2Tf[:, kc, :], Wr[:, kc, :], kc == 0, kc == 7, [h2Tf, Wr])
                    act(sc, sc[:, :], PS[2][:, 0:64], AF.Sigmoid, [PS[2]])
                    tt("dve", sbx, sbx[:, :], sc[:, :], BR[:, :], ALU.add, [sc, BR])
                    for gi_ in range(8):
                        ctx.op("dve", (lambda e, o_=m8[:, gi_, :], i_=sbx[:, gi_ * 8:(gi_ + 1) * 8]: e.max(out=o_, in_=i_)), reads=[sbx], writes=[m8])
                    tt("dve", gsc, gsc[:, :], m8[:, :, 0], m8[:, :, 1], ALU.add, [m8])
                    ctx.op("dve", (lambda e, o_=m8g[:, :], i_=gsc[:, :]: e.max(out=o_, in_=i_)), reads=[gsc], writes=[m8g])
                    ts("dve", gmk, gmk[:, :], gsc[:, :], m8g[:, 3:4], None, ALU.is_ge, None, [gsc, m8g])
                    ts("dve", gof, gof[:, :], gmk[:, :], 4.0, -4.0, ALU.mult, ALU.add, [gmk])
                    sb3v = sbx[:, :].rearrange("p (g e) -> p g e", g=8)
                    tt("dve", sbm, sbm[:, :].rearrange("p (g e) -> p g e", g=8), sb3v, gmk[:, :, None].broadcast_to([128, 8, 8]), ALU.mult, [sbx, gmk])
                    tt("dve", sbm, sbm[:, :].rearrange("p (g e) -> p g e", g=8), sbm[:, :].rearrange("p (g e) -> p g e", g=8),
                       gof[:, :, None].broadcast_to([128, 8, 8]), ALU.add, [sbm, gof])
                    ctx.op("dve", (lambda e, o_=m8e[:, :], i_=sbm[:, :]: e.max(out=o_, in_=i_)), reads=[sbm], writes=[m8e])
                    ts("dve", sel, sel[:, :], sbm[:, :], m8e[:, 7:8], None, ALU.is_ge, None, [sbm, m8e])
                    tt("dve", sel, sel[:, :], sel[:, :], sc[:, :], ALU.mult, [sel, sc])
                    red("dve", wsum, wsum[:, 0:1], sel[:, :], ALU.add, [sel])
                    recip(wsum, wsum[:, 1:2], wsum[:, 0:1], [wsum])
                    ts("dve", RW, RW[:, jj, 0:64], sel[:, :], wsum[:, 1:2], 2.5, ALU.mult, ALU.mult, [sel, wsum])
                ctx.barrier()
                ASTK.pop()
            mark(22)
            ACC = sb4("ACC", [128, NB4, D])
            mset("dve", ACC, ACC[:, :, :], 0.0)
            wge = [sb4("wge%d" % i, [128, 8, 256], BF16) for i in range(2)]
            wue = [sb4("wue%d" % i, [128, 8, 256], BF16) for i in range(2)]
            wde = [sb4("wde%d" % i, [128, 2, D], BF16) for i in range(2)]
            sgt = sb4("sgt", [128, 2, 512])
            actT = sb4("actT", [128, 2, 512], BF16)
            groups = [(t0, min(512, NTOK - t0)) for t0 in range(0, NTOK, 512)]
            for e_ in range(cfg.NE):
                c_ = e_ % 2
                ld("pool", wge[c_], wge[c_][:, :, :], w_eg[e_].rearrange("(kc p) n -> p kc n", p=128))
                ld("pool", wue[c_], wue[c_][:, :, :], w_eu[e_].rearrange("(kc p) n -> p kc n", p=128))
                ld("pool", wde[c_], wde[c_][:, :, :], w_ed[e_].rearrange("(kc p) n -> p kc n", p=128))
                for (t0, tn) in groups:
                    for fc in range(2):
                        for kc in range(8):
                            mm(PS[fc], PS[fc][:, 0:tn], wge[c_][:, kc, fc * 128:(fc + 1) * 128], H2T[:, kc, t0:t0 + tn], kc == 0, kc == 7, [wge[c_], H2T])
                        for kc in range(8):
                            mm(PS[2 + fc], PS[2 + fc][:, 0:tn], wue[c_][:, kc, fc * 128:(fc + 1) * 128], H2T[:, kc, t0:t0 + tn], kc == 0, kc == 7, [wue[c_], H2T])
                    for fc in range(2):
                        act(sgt, sgt[:, fc, 0:tn], PS[fc][:, 0:tn], AF.Silu, [PS[fc]])
                        tt("dve", actT, actT[:, fc, 0:tn], sgt[:, fc, 0:tn], PS[2 + fc][:, 0:tn], ALU.mult, [sgt, PS[2 + fc]])
                    for tb in range(tn // 128):
                        jb = (t0 // 128) + tb
                        for dh in range(2):
                            pb = PS[4 + (2 * tb + dh) % 4]
                            for fc in range(2):
                                mm(pb, pb[:, :], actT[:, fc, tb * 128:(tb + 1) * 128], wde[c_][:, fc, dh * 512:(dh + 1) * 512], fc == 0, fc == 1, [actT, wde[c_]])
                            stt("dve", ACC, ACC[:, jb, dh * 512:(dh + 1) * 512], pb[:, :], RW[:, jb, e_:e_ + 1],
                                ACC[:, jb, dh * 512:(dh + 1) * 512], ALU.mult, ALU.add, [pb, RW, ACC])
            mark(23)
            xf = [sb4("xf%d" % i, [128, D]) for i in range(2)]
            pre2 = sb4("pre2", [128, D])
            sqv2 = sb4("sqv2", [128, D])
            st5 = sb4("st5", [128, 8])
            yb = [sb4("yb%d" % i, [128, D]) for i in range(2)]
            for jj in range(NB4):
                c_ = jj % 2
                BCx = BC
                if jj == NBO and SMP:
                    mset("dve", BC, BC[:, 3, :], 0.0)
                    ld("sp", BC, BC[0:16, 3, :], modrs_d[:, 40 * 128:40 * 128 + 1024], extra_r=[modrs_res])
                ld("sp", xf[c_], xf[c_][:, :], x1s[jj * 128:(jj + 1) * 128, :], extra_r=[x1res])
                tt("dve", pre2, pre2[:, :], ACC[:, jj, :], BCx[:, 3, :], ALU.mult, [ACC, BCx])
                stt("dve", pre2, pre2[:, :], xf[c_][:, :], ALPHA, pre2[:, :], ALU.mult, ALU.add, [xf[c_], pre2])
                red("dve", st5, st5[:, 0:1], pre2[:, :], ALU.add, [pre2])
                act(sqv2, sqv2[:, :], pre2[:, :], AF.Square, [pre2])
                red("dve", st5, st5[:, 1:2], sqv2[:, :], ALU.add, [sqv2])
                ts("dve", st5, st5[:, 2:3], st5[:, 0:1], 1.0 / D, None, ALU.mult, None, [st5])
                tt("dve", st5, st5[:, 3:4], st5[:, 2:3], st5[:, 2:3], ALU.mult, [st5])
                stt("dve", st5, st5[:, 4:5], st5[:, 1:2], 1.0 / D, st5[:, 3:4], ALU.mult, ALU.subtract, [st5])
                ts("dve", st5, st5[:, 4:5], st5[:, 4:5], LN_EPS, None, ALU.add, None, [st5])
                act(st5, st5[:, 4:5], st5[:, 4:5], AF.Sqrt, [st5])
                recip(st5, st5[:, 4:5], st5[:, 4:5], [st5])
                ts("dve", sqv2, sqv2[:, :], pre2[:, :], st5[:, 2:3], None, ALU.subtract, None, [pre2, st5])
                ts("dve", sqv2, sqv2[:, :], sqv2[:, :], st5[:, 4:5], None, ALU.mult, None, [sqv2, st5])
                tt("pool", sqv2, sqv2[:, :], sqv2[:, :], LNP[:, 2, :], ALU.mult, [sqv2, LNP])
                tt("pool", yb[c_], yb[c_][:, :], sqv2[:, :], LNP[:, 3, :], ALU.add, [sqv2, LNP])
                if jj == NBO:
                    st("sp", o_ys[:, :], yb[c_], yb[c_][:, :])
                else:
                    st("sp", o_y[jj * 128:(jj + 1) * 128, :], yb[c_], yb[c_][:, :])
            ctx.barrier()
            ASTK.pop()

    ctx.dead = False
    ctx.barrier()
    ctx.op("sp", lambda e: e.nop(), reads=[], writes=[])
    ctx.emit(stack)
    stack.close()
    nc._dbg = DBG
    return nc


from concourse.bass_utils import run_bass_kernel_spmd

_NC_CACHE = {}


def rope_tab(pos):
    inv = (10000.0 ** (-np.arange(16, dtype=np.float32) / 16)).astype(np.float32)
    ang = pos.astype(np.float32)[:, None] * inv[None, :]
    cos = np.cos(ang).astype(np.float32)
    sin = np.sin(ang).astype(np.float32)
    return np.concatenate([cos, cos, -sin, sin], 1).astype(np.float32)


def kernel(**inp):
    inp = {k: np.asarray(v) for k, v in inp.items()}
    B, T, _ = inp["x_prompt"].shape
    BD = inp["x_sample"].shape[0]
    past = inp["page_table"].shape[1] * 128
    cfg = Cfg(T=T, PAST=past, NPHYS=inp["cache_ckv"].shape[1], phases=("p0", "p1", "p2", "p3", "p4"))
    cfg.debug_oa = False
    cfg.debug_ob = False
    key = (T, past)
    if key not in _NC_CACHE:
        _NC_CACHE[key] = build(cfg)
    nc = _NC_CACHE[key]
    wi = inp["w_in"][0]
    kr = wi[:, 2432:2464]
    w_kv = np.ascontiguousarray(np.concatenate([wi[:, 2176:2432], kr, kr[:, 16:32], kr[:, 0:16]], 1))
    w_rw = np.ascontiguousarray(wi[:, :1792])
    pvec = np.concatenate([inp[k][0] for k in ["w_decay0", "a0", "k_k", "k_a", "r_k", "lnx_g", "lnx_b"]])[None, :]
    ropep = rope_tab(np.arange(T))
    ropes = rope_tab(np.array([past]))
    w_g = np.ascontiguousarray(wi[:, 2464:4512])
    w_q = np.ascontiguousarray(wi[:, 1792:2176])
    wuq = inp["w_uq"][0].reshape(384, 8, 96)
    w_uqx = np.ascontiguousarray(np.concatenate([wuq.reshape(384, 768), np.concatenate([wuq[:, :, 80:96], wuq[:, :, 64:80]], 2).reshape(384, 256)], 1))
    wuk = inp["w_uk"][0].reshape(256, 8, 64)
    w_ukp = np.ascontiguousarray(np.concatenate([wuk, np.zeros((256, 8, 32), np.float32)], 2).reshape(256, 768))
    lnp = np.concatenate([inp["ln1_g"][0], inp["ln1_b"][0], inp["ln2_g"][0], inp["ln2_b"][0]])[None, :]
    w_eg = np.concatenate([inp["w_exp_gate"][0], inp["w_sh_gate"]], 0)
    w_eu = np.concatenate([inp["w_exp_up"][0], inp["w_sh_up"]], 0)
    w_ed = np.concatenate([inp["w_exp_down"][0], inp["w_sh_down"]], 0)
    kk_ = np.arange(128)[:, None]
    qq_ = np.arange(128)[None, :]
    tri = (kk_ <= qq_).astype(np.float32)
    NBO = T // 256
    cache_c = inp["cache_ckv"][0].reshape(-1, 256)
    cache_k = inp["cache_krope"][0].reshape(-1, 32)
    maps = []
    for c in range(8):
        b, hf = c // 2, c % 2
        m = dict(host_consts(hf))
        m["xp"] = inp["x_prompt"][b]
        m["cv"] = np.concatenate([inp["c_prompt"][b:b + 1], inp["c_sample"][16 * c:16 * c + 16]], 0)
        m["w_ada"] = inp["w_ada"][0]
        m["b_ada"] = inp["b_ada"][0].reshape(48, 128)
        m["w_rw"] = w_rw
        m["mu"] = inp["mu_shift"][0][None, :]
        m["pvec"] = pvec
        m["w_du"] = inp["w_decay_up"][0]
        m["w_iu"] = inp["w_iclr_up"][0]
        m["w_gu"] = inp["w_gate_up"][0]
        m["w_kv"] = w_kv
        m["gkv"] = inp["g_kvnorm"][0][None, :]
        m["ropep"] = ropep
        m["ropes"] = ropes
        m["xs"] = inp["x_sample"][16 * c:16 * c + 16, 0]
        m["st_shift"] = inp["state_shift"][0, 16 * c:16 * c + 16]
        m["st_wkv"] = inp["state_wkv"][0, 16 * c:16 * c + 16].reshape(128, 4096)
        own = np.concatenate([np.arange((2 * jj + hf) * 128, (2 * jj + hf + 1) * 128) for jj in range(NBO)])
        m["xo"] = np.ascontiguousarray(inp["x_prompt"][b][own])
        m["ropeo"] = rope_tab(own)
        m["w_q"] = w_q
        m["gq"] = inp["g_qnorm"][0][None, :]
        m["w_uqx"] = w_uqx
        m["w_ukp"] = w_ukp
        m["w_uv"] = inp["w_uv"][0]
        m["cmask"] = np.concatenate([tri, np.zeros((128, 128), np.float32)], 1) if hf == 0 else np.concatenate([np.ones((128, 128), np.float32), tri], 1)
        m["w_g"] = w_g
        m["cache_c"] = cache_c
        m["cache_k"] = cache_k
        m["ptab"] = np.ascontiguousarray(inp["page_table"][16 * c:16 * c + 16].reshape(1, -1)).astype(np.int32)
        m["w_ba"] = inp["w_branch_a"][0]
        m["w_bb"] = inp["w_branch_b"][0]
        m["w_o"] = inp["w_out"][0]
        m["lnp"] = lnp
        m["w_r"] = inp["w_router"][0]
        m["b_r"] = inp["b_router"][0][None, :]
        m["w_eg"] = w_eg
        m["w_eu"] = w_eu
        m["w_ed"] = w_ed
        maps.append(m)
    res = run_bass_kernel_spmd(nc, maps, core_ids=list(range(8))).results
    f32 = np.float32
    y_prompt = np.zeros((B, T, 1024), f32)
    for c in range(8):
        b, hf = c // 2, c % 2
        oy = res[c]["o_y"]
        for jj in range(NBO):
            y_prompt[b, (2 * jj + hf) * 128:(2 * jj + hf + 1) * 128] = oy[jj * 128:(jj + 1) * 128]
    y_sample = np.concatenate([res[c]["o_ys"][0:16] for c in range(8)], 0)[:, None, :].astype(f32)
    ckv_p = np.stack([res[2 * b]["o_ckv"] for b in range(B)])[None].astype(f32)
    kr_p = np.stack([res[2 * b]["o_kr"] for b in range(B)])[None].astype(f32)
    wkv_p = np.stack([res[2 * b]["o_wkv"] for b in range(B)])[None].astype(f32)
    sh_p = np.stack([res[2 * b]["o_shift"][0] for b in range(B)])[None].astype(f32)
    smp = np.concatenate([res[c]["o_smp"] for c in range(8)], 0)
    ckv_s = np.ascontiguousarray(smp[:, 0:256])[None, :, None, :].astype(f32)
    kr_s = np.ascontiguousarray(smp[:, 256:288])[None, :, None, :].astype(f32)
    wkv_s = np.concatenate([res[c]["o_wkv_s"].reshape(16, 8, 64, 64) for c in range(8)], 0)[None].astype(f32)
    sh_s = np.concatenate([res[c]["o_shift_s"] for c in range(8)], 0)[None].astype(f32)
    return (y_prompt, y_sample, ckv_p, kr_p, wkv_p, sh_p, ckv_s, kr_s, wkv_s, sh_s)
```
